# Optimizing a Trainium2 kernel written in Bass

```python
import math
import jax, jax.numpy as jnp
from jax import lax
import numpy as np

D_MODEL = 1024
BATCH = 4
SEQ = 4096
DEPTH = 2

HEAD_DIM = 64
V_HEAD_DIM = 2 * HEAD_DIM
ATTN_WIDTH = D_MODEL // 2
ATTN_HEADS = ATTN_WIDTH // V_HEAD_DIM
QK_WIDTH = ATTN_HEADS * 2 * HEAD_DIM
Q_BLOCK = 128
ROPE_THETA = 10000.0
LRU_WIDTH = D_MODEL // 2
LRU_BLOCKS = 8
LRU_BLOCK = LRU_WIDTH // LRU_BLOCKS
LRU_C = 8.0
CONV_WIDTH = 4
CONV_PAD = (2, 1)
D_FF = ((8 * D_MODEL // 3 + 255) // 256) * 256
N_EXPERTS = 8
TOP_K = 2
D_FF_EXPERT = 7 * D_MODEL // 2
N_DENSE = (DEPTH + 1) // 2
N_MOE = DEPTH // 2
DEEPNORM_ALPHA = (2.0 * DEPTH) ** 0.25
DEEPNORM_BETA = (8.0 * DEPTH) ** -0.25
LN_EPS = 1e-5
IN_SPLITS = [QK_WIDTH, 2 * QK_WIDTH, 2 * QK_WIDTH + ATTN_WIDTH,
             2 * QK_WIDTH + ATTN_WIDTH + LRU_WIDTH,
             2 * QK_WIDTH + ATTN_WIDTH + 2 * LRU_WIDTH,
             2 * QK_WIDTH + ATTN_WIDTH + 2 * LRU_WIDTH + D_MODEL]
IN_COLS = 2 * QK_WIDTH + ATTN_WIDTH + 2 * LRU_WIDTH + 2 * D_MODEL

kernel_name = "hybrid_diffattn_rglru_moe_encoder"


def layernorm(x, g, b):
    xf = x.astype(jnp.float32)
    mu = jnp.mean(xf, axis=-1, keepdims=True)
    var = jnp.mean(jnp.square(xf - mu), axis=-1, keepdims=True)
    y = (xf - mu) * lax.rsqrt(var + LN_EPS)
    return (y * g.astype(jnp.float32) + b.astype(jnp.float32)).astype(x.dtype)


def rmsnorm(x, g):
    xf = x.astype(jnp.float32)
    y = xf * lax.rsqrt(jnp.mean(jnp.square(xf), axis=-1, keepdims=True) + LN_EPS)
    return (y * g.astype(jnp.float32)).astype(x.dtype)


def rotary(t, pos):
    half = HEAD_DIM // 2
    inv = 1.0 / (ROPE_THETA ** (jnp.arange(half, dtype=jnp.float32) * (2.0 / HEAD_DIM)))
    ang = pos.astype(jnp.float32)[:, None] * inv[None, :]
    cos = jnp.cos(ang)[None, :, None, None, :].astype(t.dtype)
    sin = jnp.sin(ang)[None, :, None, None, :].astype(t.dtype)
    t1, t2 = t[..., :half], t[..., half:]
    return jnp.concatenate([t1 * cos - t2 * sin, t2 * cos + t1 * sin], axis=-1)


def diff_attention(q, k, v, lam):
    B, S = q.shape[0], q.shape[1]
    nblk = S // Q_BLOCK
    qb = (q * (HEAD_DIM ** -0.5)).reshape(B, nblk, Q_BLOCK, ATTN_HEADS, 2, HEAD_DIM)
    qb = jnp.moveaxis(qb, 1, 0)

    def one_block(qblk):
        s = jnp.einsum('bqhmd,bkhmd->bhmqk', qblk, k).astype(jnp.float32)
        p = jax.nn.softmax(s, axis=-1)
        w = p[:, :, 0] - lam * p[:, :, 1]
        return jnp.einsum('bhqk,bkhe->bqhe', w.astype(v.dtype), v)

    out = lax.map(one_block, qb)
    return jnp.moveaxis(out, 0, 1).reshape(B, S, ATTN_HEADS, V_HEAD_DIM)


def linear_scan(a, b, reverse):
    def comb(l, r):
        return (l[0] * r[0], r[0] * l[1] + r[1])
    _, h = lax.associative_scan(comb, (a, b), axis=1, reverse=reverse)
    return h


def rg_lru(xc, wa, ba, wx, bx, lam, reverse):
    B, S, C = xc.shape
    xb = xc.reshape(B, S, LRU_BLOCKS, LRU_BLOCK)
    r = jax.nn.sigmoid(jnp.einsum('bsnc,ncd->bsnd', xb, wa).reshape(B, S, C) + ba)
    i = jax.nn.sigmoid(jnp.einsum('bsnc,ncd->bsnd', xb, wx).reshape(B, S, C) + bx)
    log_a = -LRU_C * r.astype(jnp.float32) * jax.nn.softplus(-lam.astype(jnp.float32))
    a = jnp.exp(log_a)
    b = jnp.sqrt(-jnp.expm1(2.0 * log_a)) * (i * xc).astype(jnp.float32)
    return linear_scan(a, b, reverse).astype(xc.dtype)


def depthwise_conv(x, w, b):
    C = x.shape[-1]
    y = lax.conv_general_dilated(x, w[:, None, :].astype(x.dtype), window_strides=(1,),
                                 padding=[CONV_PAD], dimension_numbers=('NWC', 'WIO', 'NWC'),
                                 feature_group_count=C)
    return y + b


def swiglu(h, wg, wu, wd):
    return (jax.nn.silu(h @ wg) * (h @ wu)) @ wd


def moe_swiglu(h, wr, br, wg, wu, wd):
    B, S, D = h.shape
    ht = h.reshape(B * S, D)
    logits = (ht @ wr + br).astype(jnp.float32)
    top_v, top_i = lax.top_k(logits, TOP_K)
    gates = jax.nn.softmax(top_v, axis=-1)
    combine = jnp.sum(jax.nn.one_hot(top_i, N_EXPERTS, dtype=jnp.float32) * gates[..., None], axis=1)
    combine = combine.astype(h.dtype)
    y = jnp.zeros_like(ht)
    for e in range(N_EXPERTS):
        y = y + combine[:, e:e + 1] * swiglu(ht, wg[e], wu[e], wd[e])
    return y.reshape(B, S, D)


def setup_inputs(seed: int = 0) -> dict:
    key = jax.random.key(seed)
    keys = iter(jax.random.split(key, 64))

    def nrm(shape, scale):
        return jax.random.normal(next(keys), shape, jnp.float32) * scale

    def gain(shape):
        return 1.0 + nrm(shape, 0.02)

    L = DEPTH
    C = LRU_WIDTH
    u = jax.random.uniform(next(keys), (L, 2, C), jnp.float32, 0.9, 0.999)
    a0 = u ** (1.0 / LRU_C)
    rg_lam = jnp.log(a0) - jnp.log1p(-a0)
    beta = DEEPNORM_BETA
    return {
        "x": nrm((BATCH, SEQ, D_MODEL), 1.0),
        "ln_in_g": gain((D_MODEL,)),
        "ln_in_b": nrm((D_MODEL,), 0.02),
        "w_in": nrm((L, D_MODEL, IN_COLS), D_MODEL ** -0.5),
        "b_in": nrm((L, IN_COLS), 0.01),
        "lam_q1": nrm((L, HEAD_DIM), 0.1),
        "lam_k1": nrm((L, HEAD_DIM), 0.1),
        "lam_q2": nrm((L, HEAD_DIM), 0.1),
        "lam_k2": nrm((L, HEAD_DIM), 0.1),
        "subln_g": gain((L, V_HEAD_DIM)),
        "conv_w": nrm((L, CONV_WIDTH, C), CONV_WIDTH ** -0.5),
        "conv_b": nrm((L, C), 0.01),
        "rg_wa": nrm((L, 2, LRU_BLOCKS, LRU_BLOCK, LRU_BLOCK), LRU_BLOCK ** -0.5),
        "rg_ba": nrm((L, 2, C), 0.01),
        "rg_wx": nrm((L, 2, LRU_BLOCKS, LRU_BLOCK, LRU_BLOCK), LRU_BLOCK ** -0.5),
        "rg_bx": nrm((L, 2, C), 0.01),
        "rg_lam": rg_lam,
        "w_pa": nrm((L, ATTN_WIDTH, D_MODEL), ATTN_WIDTH ** -0.5 * beta),
        "w_pb": nrm((L, LRU_WIDTH, D_MODEL), LRU_WIDTH ** -0.5 * beta),
        "w_o": nrm((L, D_MODEL, D_MODEL), D_MODEL ** -0.5 * beta),
        "b_o": nrm((L, D_MODEL), 0.01),
        "ln1_g": gain((L, D_MODEL)),
        "ln1_b": nrm((L, D_MODEL), 0.02),
        "ffn_wg": nrm((N_DENSE, D_MODEL, D_FF), D_MODEL ** -0.5),
        "ffn_wu": nrm((N_DENSE, D_MODEL, D_FF), D_MODEL ** -0.5 * beta),
        "ffn_wd": nrm((N_DENSE, D_FF, D_MODEL), D_FF ** -0.5 * beta),
        "moe_wr": nrm((N_MOE, D_MODEL, N_EXPERTS), D_MODEL ** -0.5),
        "moe_br": nrm((N_MOE, N_EXPERTS), 0.01),
        "moe_wg": nrm((N_MOE, N_EXPERTS, D_MODEL, D_FF_EXPERT), D_MODEL ** -0.5),
        "moe_wu": nrm((N_MOE, N_EXPERTS, D_MODEL, D_FF_EXPERT), D_MODEL ** -0.5 * beta),
        "moe_wd": nrm((N_MOE, N_EXPERTS, D_FF_EXPERT, D_MODEL), D_FF_EXPERT ** -0.5 * beta),
        "ln2_g": gain((L, D_MODEL)),
        "ln2_b": nrm((L, D_MODEL), 0.02),
    }


def reference(x, ln_in_g, ln_in_b, w_in, b_in, lam_q1, lam_k1, lam_q2, lam_k2, subln_g,
              conv_w, conv_b, rg_wa, rg_ba, rg_wx, rg_bx, rg_lam, w_pa, w_pb, w_o, b_o,
              ln1_g, ln1_b, ffn_wg, ffn_wu, ffn_wd, moe_wr, moe_br, moe_wg, moe_wu, moe_wd,
              ln2_g, ln2_b):
    B, S, _ = x.shape
    pos = jnp.arange(S, dtype=jnp.int32)
    h = layernorm(x, ln_in_g, ln_in_b)
    for l in range(DEPTH):
        proj = h @ w_in[l] + b_in[l]
        q, k, v, xr, gr, ga, gb = jnp.split(proj, IN_SPLITS, axis=-1)
        q = rotary(q.reshape(B, S, ATTN_HEADS, 2, HEAD_DIM), pos)
        k = rotary(k.reshape(B, S, ATTN_HEADS, 2, HEAD_DIM), pos)
        v = v.reshape(B, S, ATTN_HEADS, V_HEAD_DIM)
        lambda_init = 0.8 - 0.6 * math.exp(-0.3 * l)
        lam = (jnp.exp(jnp.sum(lam_q1[l].astype(jnp.float32) * lam_k1[l].astype(jnp.float32)))
               - jnp.exp(jnp.sum(lam_q2[l].astype(jnp.float32) * lam_k2[l].astype(jnp.float32)))
               + lambda_init)
        att = diff_attention(q, k, v, lam)
        att = rmsnorm(att, subln_g[l]) * (1.0 - lambda_init)
        branch_a = att.reshape(B, S, ATTN_WIDTH) @ w_pa[l]
        xc = depthwise_conv(xr, conv_w[l], conv_b[l])
        h_fwd = rg_lru(xc, rg_wa[l, 0], rg_ba[l, 0], rg_wx[l, 0], rg_bx[l, 0], rg_lam[l, 0], False)
        h_bwd = rg_lru(xc, rg_wa[l, 1], rg_ba[l, 1], rg_wx[l, 1], rg_bx[l, 1], rg_lam[l, 1], True)
        branch_b = ((h_fwd + h_bwd) * jax.nn.gelu(gr)) @ w_pb[l]
        merged = jax.nn.sigmoid(ga) * branch_a + jax.nn.sigmoid(gb) * branch_b
        m = merged @ w_o[l] + b_o[l]
        h = layernorm(DEEPNORM_ALPHA * h + m, ln1_g[l], ln1_b[l])
        if l % 2 == 0:
            f = swiglu(h, ffn_wg[l // 2], ffn_wu[l // 2], ffn_wd[l // 2])
        else:
            j = l // 2
            f = moe_swiglu(h, moe_wr[j], moe_br[j], moe_wg[j], moe_wu[j], moe_wd[j])
        h = layernorm(DEEPNORM_ALPHA * h + f, ln2_g[l], ln2_b[l])
    return h
```

```python
import math
from contextlib import ExitStack

import numpy as np
import concourse.bass as bass
import concourse.mybir as mybir
from concourse.bass_utils import run_bass_kernel_spmd

F32 = mybir.dt.float32
BF16 = mybir.dt.bfloat16
AF = mybir.ActivationFunctionType
ALU = mybir.AluOpType
AX = mybir.AxisListType

ENGS = ("pe", "act", "dve", "pool", "sp")
N_DMA_SLOTS = 8

D = 1024
T = 4096
TH = 2048
L = 2
NE = 8
DFF = 2816
DFE = 3584
ALPHA = (2.0 * L) ** 0.25
EPS = 1e-5
NCOLP = 44 + 5 * 4 + 4 + 8 + 8 + 8
BCL = 4 * 1024 + 1024 + 512 + 128 + 256 + 8


class Res:
    __slots__ = ("name", "writers", "readers", "multi", "ignore")

    def __init__(self, name, multi=False, ignore=False):
        self.name = name
        self.writers = []
        self.readers = []
        self.multi = multi
        self.ignore = ignore


class Op:
    __slots__ = ("eng", "fn", "deps", "dma", "slot", "sig", "needed", "idx")


class Prog:
    def __init__(self, nc, same_engine_sync=("act", "dve", "pool")):
        self.nc = nc
        self.ops = []
        self.same = set(same_engine_sync)
        self.dma_count = {"sp": 0, "pool": 0, "act": 0}
        self.last_eng = {}
        self.last_slot = {}

    def res(self, name, multi=False, ignore=False):
        return Res(name, multi, ignore)

    def op(self, eng, fn, reads=(), writes=(), dma=False, extra_deps=()):
        o = Op()
        o.eng, o.fn, o.dma = eng, fn, dma
        o.idx = len(self.ops)
        deps = set(extra_deps)
        reads = [r for r in reads if not r.ignore]
        writes = [w for w in writes if not w.ignore]
        for r in reads:
            deps.update(r.writers)
        for w in writes:
            deps.update(w.readers)
            if not (w.multi and not w.readers):
                deps.update(w.writers)
        for w in writes:
            if w.multi and not w.readers:
                w.writers.append(o.idx)
            else:
                w.writers = [o.idx]
            w.readers = []
        for r in reads:
            if r not in writes:
                r.readers.append(o.idx)
        o.deps = deps
        o.needed = False
        o.slot = None
        o.sig = None
        if dma:
            o.slot = self.dma_count[eng] % N_DMA_SLOTS
            self.dma_count[eng] += 1
            self.last_slot[(eng, o.slot)] = o.idx
        elif fn is not None:
            self.last_eng[eng] = o.idx
        self.ops.append(o)
        return o

    def dma(self, out, in_, reads=(), writes=(), eng="sp"):
        return self.op(eng, lambda e: e.dma_start(out=out, in_=in_), reads, writes, dma=True)

    def barrier(self):
        tails = list(self.last_eng.values()) + list(self.last_slot.values())
        for eng in ENGS:
            self.op(eng, None, extra_deps=tails)

    def finalize(self, stack):
        nc = self.nc
        ops = self.ops
        for o in ops:
            keep = set()
            for d in o.deps:
                p = ops[d]
                if p.fn is None:
                    continue
                if (not p.dma) and p.eng == o.eng and (o.eng not in self.same) and not o.dma:
                    continue
                if (not p.dma) and p.eng == o.eng and o.fn is None:
                    continue
                keep.add(d)
            o.deps = keep
            for d in keep:
                ops[d].needed = True
        esem = {e: stack.enter_context(nc.semaphore("sem_" + e)) for e in ENGS}
        dsem = {}
        for q in ("sp", "pool", "act"):
            if self.dma_count[q]:
                dsem[q] = [stack.enter_context(nc.semaphore("dsem_%s%d" % (q, i)))
                           for i in range(N_DMA_SLOTS)]
        ecount = {e: 0 for e in ENGS}
        dcount = {q: [0] * N_DMA_SLOTS for q in dsem}
        for o in ops:
            if o.dma:
                c = dcount[o.eng]
                c[o.slot] += 1
                o.sig = (dsem[o.eng][o.slot], 16 * c[o.slot], 16)
            elif o.needed:
                ecount[o.eng] += 1
                o.sig = (esem[o.eng], ecount[o.eng], 1)
        self.max_counts = dict(ecount)
        per_eng = {e: [] for e in ENGS}
        seen = {e: {} for e in ENGS}
        for o in ops:
            waits = []
            cand = {}
            for d in o.deps:
                s, v, _ = ops[d].sig
                k = id(s)
                if k not in cand or cand[k][1] < v:
                    cand[k] = (s, v)
            if o.dma:
                s, v, _ = o.sig
                if v > 16:
                    k = id(s)
                    if k not in cand or cand[k][1] < v - 16:
                        cand[k] = (s, v - 16)
            for k, (s, v) in cand.items():
                if seen[o.eng].get(k, 0) >= v:
                    continue
                seen[o.eng][k] = v
                waits.append((s, v))
            per_eng[o.eng].append((o, waits))
        self.per_eng = per_eng
        self.final_sigs = {}
        for o in ops:
            if o.sig is not None:
                self.final_sigs[id(o.sig[0])] = (o.sig[0], o.sig[1])

    def emit(self, block, final_wait_eng="sp"):
        per_eng = self.per_eng
        finals = list(self.final_sigs.values())

        def run(eng_name):
            def body(e):
                for o, waits in per_eng[eng_name]:
                    for s, v in waits:
                        e.wait_ge(s, v)
                    if o.fn is None:
                        continue
                    ins = o.fn(e)
                    if o.sig is not None:
                        ins.then_inc(o.sig[0], o.sig[2])
                if eng_name == final_wait_eng:
                    for s, v in finals:
                        e.wait_ge(s, v)
            return body

        block.tensor(run("pe"))
        block.scalar(run("act"))
        block.vector(run("dve"))
        block.gpsimd(run("pool"))
        block.sync(run("sp"))


class Arena:
    def __init__(self, t, n_f32):
        self.t = t
        self.n = n_f32
        self.off = 0

    def mark(self):
        return self.off

    def release(self, m):
        self.peak = max(getattr(self, "peak", 0), self.off)
        self.off = m

    def f32(self, n):
        n4 = (n + 7) // 8 * 8
        assert self.off + n4 <= self.n, ("arena overflow", self.off, n4, self.n)
        ap = self.t[:, self.off:self.off + n]
        self.off += n4
        return ap

    def bf16(self, n):
        nf = (n + 1) // 2
        nf = (nf + 7) // 8 * 8
        assert self.off + nf <= self.n, ("arena overflow", self.off, nf, self.n)
        ap = self.t[:, self.off:self.off + nf].bitcast(BF16)[:, 0:n]
        self.off += nf
        return ap


def build_program(upto=99, debug=False):
    nc = bass.Bass("TRN2", target_bir_lowering=False)
    okind = "ExternalOutput" if debug else "Internal"

    def din(name, shape, dt=F32):
        return nc.dram_tensor(name, list(shape), dt, kind="ExternalInput").ap()

    x_d = din("x", [T, D])
    bc0_d = din("bc0", [128, 2 * D])
    bcl_d = din("bcl", [L, 128, BCL])
    colp_d = din("colp", [128, L * NCOLP])
    cs_d = din("cs", [128, 2, T])
    ident_d = din("ident", [128, 128])
    win_d = din("w_in_ext", [L, D, 5632])
    rgbd_d = din("rg_bd", [L, 16, 128, 128])
    wpa_d = din("w_pa", [L, 512, D])
    wpb_d = din("w_pb", [L, 512, D])
    wo_d = din("w_o", [L, D, D])
    fwg_d = din("ffn_wg", [1, D, DFF])
    fwu_d = din("ffn_wu", [1, D, DFF])
    fwd_d = din("ffn_wd", [1, DFF, D])
    mwr_d = din("moe_wr", [1, D, NE])
    mwg_d = din("moe_wg", [1, NE, D, DFE])
    mwu_d = din("moe_wu", [1, NE, D, DFE])
    mwd_d = din("moe_wd", [1, NE, DFE, D])

    out_d = nc.dram_tensor("out", [TH, D], F32, kind="ExternalOutput").ap()
    hres_d = nc.dram_tensor("hres", [T, D], F32, kind=okind).ap()
    hT_d = nc.dram_tensor("hT", [D, T], BF16, kind=okind).ap()
    xr_d = nc.dram_tensor("xr", [512, T], F32, kind=okind).ap()
    gates_d = nc.dram_tensor("gates", [2560, T], BF16, kind=okind).ap()
    yb_d = nc.dram_tensor("yb", [512, T], BF16, kind=okind).ap()
    att_d = nc.dram_tensor("att", [512, T], BF16, kind=okind).ap()
    dbg = {}
    if debug:
        dbg["qT"] = nc.dram_tensor("dbg_qT", [128, 4 * T], BF16, kind="ExternalOutput").ap()
        dbg["kT"] = nc.dram_tensor("dbg_kT", [128, 4 * T], BF16, kind="ExternalOutput").ap()
        dbg["v"] = nc.dram_tensor("dbg_v", [128, 32 * 4 * 130], BF16, kind="ExternalOutput").ap()
        dbg["acc"] = nc.dram_tensor("dbg_acc", [128, 4 * 512], F32, kind="ExternalOutput").ap()
        dbg["pt"] = nc.dram_tensor("dbg_pt", [128, 1024], BF16, kind="ExternalOutput").ap()
        dbg["otok"] = nc.dram_tensor("dbg_otok", [128, 2048], BF16, kind="ExternalOutput").ap()

    with ExitStack() as st:
        NAR = 51 * 1024 + 512
        arena_t = st.enter_context(nc.sbuf_tensor("arena", [128, NAR], F32))
        ident = st.enter_context(nc.sbuf_tensor("ident_sb", [128, 128], BF16))
        colp = st.enter_context(nc.sbuf_tensor("colp_sb", [128, L * NCOLP], F32))
        small = st.enter_context(nc.sbuf_tensor("small", [128, 64], F32))
        ps = st.enter_context(nc.psum_tensor("ps", [128, 8, 512], F32))
        A = Arena(arena_t, NAR)
        P = Prog(nc)
        r_ps = [P.res("ps%d" % i) for i in range(8)]
        r_ident = P.res("ident")
        r_colp = P.res("colp")
        r_small = P.res("small")
        r_hres, r_hT, r_xr, r_gates, r_yb, r_att, r_out = [P.res(n, ignore=True) for n in
                                                           "hres hT xr gates yb att out".split()]

        P.dma(ident[:], ident_d, writes=[r_ident], eng="pool")
        P.dma(colp[:], colp_d, writes=[r_colp])

        def psb(i):
            return ps[:, i, :]

        def psb16(i):
            return ps[:, i, :].bitcast(BF16)

        def mm(out, lhsT, rhs, start, stop, reads, writes, sgc=False):
            if sgc:
                P.op("pe", lambda e: e.matmul(out, lhsT, rhs, start=start, stop=stop, skip_group_check=True), reads, writes)
            else:
                P.op("pe", lambda e: e.matmul(out, lhsT, rhs, start=start, stop=stop), reads, writes)

        def actf(out, in_, func, reads, writes, bias=None, scale=None):
            kw = {}
            if bias is not None:
                kw["bias"] = bias
            if scale is not None:
                kw["scale"] = scale
            P.op("act", lambda e: e.activation(out=out, in_=in_, func=func, **kw), reads, writes)

        def tt(eng, out, in0, in1, op, reads, writes):
            P.op(eng, lambda e: e.tensor_tensor(out, in0, in1, op), reads, writes)

        def ts(eng, out, in0, s1, s2, op0, op1, reads, writes):
            if op1 is None:
                P.op(eng, lambda e: e.tensor_scalar(out, in0, s1, None, op0), reads, writes)
            else:
                P.op(eng, lambda e: e.tensor_scalar(out, in0, s1, s2, op0, op1), reads, writes)

        def stt(eng, out, in0, scalar, in1, op0, op1, reads, writes):
            P.op(eng, lambda e: e.scalar_tensor_tensor(out, in0, scalar, in1, op0, op1), reads, writes)

        def memset(eng, ap, val, writes):
            P.op(eng, lambda e: e.memset(ap, val), (), writes)

        def cp(l, i):
            return colp[:, l * NCOLP + i:l * NCOLP + i + 1]

        ln_ctr = [0]

        def layernorm(src, r_src, dst, r_dst, g_bc, b_bc, r_bc, tmp, r_tmp, front_only=False, eps=EPS):
            k = ln_ctr[0] % 2
            ln_ctr[0] += 1
            stats = small[:, k * 16:k * 16 + 12]
            mv = small[:, k * 16 + 12:k * 16 + 14]
            rstd = small[:, k * 16 + 14:k * 16 + 15]
            nmr = small[:, k * 16 + 15:k * 16 + 16]
            rs = r_smallk[k]
            P.op("dve", lambda e: e.bn_stats(stats[:, 0:6], src[:, 0:512]), [r_src], [rs])
            P.op("dve", lambda e: e.bn_stats(stats[:, 6:12], src[:, 512:1024]), [r_src], [rs])
            P.op("dve", lambda e: e.bn_aggr(mv, stats), [rs], [rs])
            actf(rstd, mv[:, 1:2], AF.Sqrt, [rs], [rs], bias=eps)
            P.op("dve", lambda e: e.reciprocal(rstd, rstd), [rs], [rs])
            stt("dve", nmr, mv[:, 0:1], -1.0, rstd, ALU.mult, ALU.mult, [rs], [rs])
            actf(tmp, src, AF.Identity, [r_src, rs], [r_tmp], bias=nmr, scale=rstd)
            tt("dve", tmp, tmp, g_bc, ALU.mult, [r_tmp, r_bc], [r_tmp])
            if not front_only:
                ln_back(dst, r_dst, b_bc, r_bc, tmp, r_tmp)

        def ln_back(dst, r_dst, b_bc, r_bc, tmp, r_tmp):
            tt("dve", dst, tmp, b_bc, ALU.add, [r_tmp, r_bc], [r_dst])

        r_smallk = [P.res("small0"), P.res("small1"), P.res("small2"), P.res("small3")]

        def emit_h(tok0, hblk, r_hblk, xbf, r_xbf, hTs, r_hTs, j, nblk_stage, pbank, write_hres=True):
            if write_hres:
                P.dma(hres_d[tok0:tok0 + 128, :], hblk, reads=[r_hblk], writes=[r_hres])
            actf(xbf, hblk, AF.Copy, [r_hblk], [r_xbf])
            for i2 in range(2):
                bank = pbank[i2]
                pb = psb(bank).rearrange("p (a b) -> p a b", b=128)
                for k4 in range(4):
                    kc = i2 * 4 + k4
                    mm(pb[:, k4, :], xbf[:, kc * 128:(kc + 1) * 128], ident[:], True, True, [r_xbf, r_ident], [r_ps[bank]])
                P.op("dve", lambda e, pb=pb, i2=i2: e.tensor_copy(hTs[:, i2 * 4:(i2 + 1) * 4, j * 128:(j + 1) * 128], pb),
                     [r_ps[bank]], [r_hTs])
            if j == nblk_stage - 1:
                t0 = tok0 - j * 128
                P.dma(hT_d[:, t0:t0 + nblk_stage * 128].rearrange("(kc p) n -> p kc n", p=128), hTs,
                      reads=[r_hTs], writes=[r_hT])

        class LNPipe:
            def __init__(self):
                self.q = []
                self.n = 0

            def push(self, job):
                job["slot"] = self.n % 4
                self.n += 1
                self.q.insert(0, job)
                self._step()

            def _step(self):
                q = self.q
                if len(q) > 4 and q[4] is not None:
                    self.phE(q[4])
                if len(q) > 3 and q[3] is not None:
                    self.phD(q[3])
                if len(q) > 2 and q[2] is not None:
                    self.phC(q[2])
                if len(q) > 1 and q[1] is not None:
                    self.phB(q[1])
                if len(q) > 0 and q[0] is not None:
                    self.phA(q[0])
                if len(q) > 4:
                    q.pop()

            def flush(self):
                for _ in range(4):
                    self.q.insert(0, None)
                    self._step()
                self.q = []

            @staticmethod
            def _sm(job):
                k = job["slot"]
                return (small[:, k * 16:k * 16 + 12], small[:, k * 16 + 12:k * 16 + 14],
                        small[:, k * 16 + 14:k * 16 + 15], small[:, k * 16 + 15:k * 16 + 16], r_smallk[k])

            def phA(self, j):
                stats, mv, rstd, nmr, rs = self._sm(j)
                src, r_src = j["src"], j["r_src"]
                P.op("dve", lambda e: e.bn_stats(stats[:, 0:6], src[:, 0:512]), [r_src], [rs])
                P.op("dve", lambda e: e.bn_stats(stats[:, 6:12], src[:, 512:1024]), [r_src], [rs])
                P.op("dve", lambda e: e.bn_aggr(mv, stats), [rs], [rs])
                actf(rstd, mv[:, 1:2], AF.Sqrt, [rs], [rs], bias=j.get("eps", EPS))

            def phB(self, j):
                stats, mv, rstd, nmr, rs = self._sm(j)
                P.op("dve", lambda e: e.reciprocal(rstd, rstd), [rs], [rs])
                stt("dve", nmr, mv[:, 0:1], -1.0, rstd, ALU.mult, ALU.mult, [rs], [rs])
                actf(j["tmp"], j["src"], AF.Identity, [j["r_src"], rs], [j["r_tmp"]], bias=nmr, scale=rstd)

            def phC(self, j):
                tmp, r_tmp, dst, r_dst = j["tmp"], j["r_tmp"], j["dst"], j["r_dst"]
                tt("dve", tmp, tmp, j["g"], ALU.mult, [r_tmp, j["r_bc"]], [r_tmp])
                tt("dve", dst, tmp, j["b"], ALU.add, [r_tmp, j["r_bc"]], [r_dst])
                tok0 = j["tok0"]
                if j["final"]:
                    P.dma(out_d[tok0:tok0 + 128, :], dst, reads=[r_dst], writes=[r_out])
                    return
                P.dma(hres_d[tok0:tok0 + 128, :], dst, reads=[r_dst], writes=[r_hres])
                xbf, r_xbf = j["xbf"], j["r_xbf"]
                actf(xbf, dst, AF.Copy, [r_dst], [r_xbf])

            def phD(self, j):
                if j["final"]:
                    return
                xbf, r_xbf = j["xbf"], j["r_xbf"]
                for i2 in range(2):
                    bank = j["pbank"][i2]
                    pb = psb(bank).rearrange("p (a b) -> p a b", b=128)
                    for k4 in range(4):
                        kc = i2 * 4 + k4
                        mm(pb[:, k4, :], xbf[:, kc * 128:(kc + 1) * 128], ident[:], True, True, [r_xbf, r_ident], [r_ps[bank]])

            def phE(self, j):
                if j["final"]:
                    return
                hTs, r_hTs, jj = j["hTs"], j["r_hTs"], j["j"]
                for i2 in range(2):
                    bank = j["pbank"][i2]
                    pb = psb(bank).rearrange("p (a b) -> p a b", b=128)
                    P.op("act", lambda e, pb=pb, i2=i2: e.activation(out=hTs[:, i2 * 4:(i2 + 1) * 4, jj * 128:(jj + 1) * 128], in_=pb,
                                                                  func=AF.Copy), [r_ps[bank]], [r_hTs])
                if jj == 3:
                    t0 = j["tok0"] - jj * 128
                    P.dma(hT_d[:, t0:t0 + 512].rearrange("(kc p) n -> p kc n", p=128), hTs, reads=[r_hTs], writes=[r_hT])

        def stage0():
            m0 = A.mark()
            bc0 = A.f32(2 * D)
            r_bc0 = P.res("bc0")
            P.dma(bc0, bc0_d, writes=[r_bc0])
            xt = [A.f32(D) for _ in range(4)]
            r_xt = [P.res("xt%d" % i) for i in range(4)]
            tmp = [A.f32(D) for _ in range(2)]
            r_tmp = [P.res("tmp%d" % i) for i in range(2)]
            ht = [A.f32(D) for _ in range(2)]
            r_ht = [P.res("ht%d" % i) for i in range(2)]
            xbf = [A.bf16(D) for _ in range(2)]
            r_xbf = [P.res("xbf%d" % i) for i in range(2)]
            hTs = [A.bf16(8 * 512).rearrange("p (a b) -> p a b", b=512) for _ in range(2)]
            r_hTs = [P.res("hTs%d" % i, multi=True) for i in range(2)]
            pipe = LNPipe()
            for pb_ in range(2):
                P.dma(xt[pb_], x_d[pb_ * 128:(pb_ + 1) * 128, :], writes=[r_xt[pb_]])
            for blk in range(T // 128):
                k = blk % 2
                x4 = blk % 4
                nb_ = blk + 2
                if nb_ < T // 128:
                    P.dma(xt[nb_ % 4], x_d[nb_ * 128:(nb_ + 1) * 128, :], writes=[r_xt[nb_ % 4]])
                sg_ = (blk // 4) % 2
                pipe.push(dict(src=xt[x4], r_src=r_xt[x4], dst=ht[k], r_dst=r_ht[k], g=bc0[:, 0:D], b=bc0[:, D:2 * D], r_bc=r_bc0,
                               tmp=tmp[k], r_tmp=r_tmp[k], tok0=blk * 128, final=False, xbf=xbf[k], r_xbf=r_xbf[k],
                               hTs=hTs[sg_], r_hTs=r_hTs[sg_], j=blk % 4, pbank=(2 * k, 2 * k + 1)))
            pipe.flush()
            P.barrier()
            A.release(m0)

        def load_bcl(l):
            bcl = A.f32(BCL)
            r = P.res("bcl")
            P.dma(bcl, bcl_d[l], writes=[r])
            return bcl, r

        def stage1(l, qT, kT, vsb, r_qT, r_kT, r_v):
            m0 = A.mark()
            bcl, r_bcl = load_bcl(l)
            hTh = A.bf16(8 * TH).rearrange("p (a b) -> p a b", b=TH)
            W = [A.bf16(8 * 512).rearrange("p (a b) -> p a b", b=512) for _ in range(2)]
            r_W = [P.res("W%d" % i) for i in range(2)]
            cst = [A.f32(2 * 512).rearrange("p (a b) -> p a b", b=512) for _ in range(2)]
            r_cst = [P.res("cst%d" % i) for i in range(2)]
            t1 = [A.f32(512) for _ in range(2)]
            t2 = [A.f32(512) for _ in range(2)]
            r_t1 = [P.res("t1_%d" % i) for i in range(2)]
            r_t2 = [P.res("t2_%d" % i) for i in range(2)]
            xs = [A.f32(512) for _ in range(2)]
            r_xs = [P.res("xs%d" % i) for i in range(2)]
            gs = [A.bf16(4 * 512).rearrange("p (a b) -> p a b", b=512) for _ in range(2)]
            r_gs = [P.res("gs%d" % i, multi=True) for i in range(2)]
            memset("pool", vsb[:, :, :, 128:130], 1.0, [r_v])
            bv_bc = bcl[:, 5 * 1024:5 * 1024 + 512].rearrange("p (h e) -> p h e", e=128)
            wi = [0]
            ci = [0]
            xi = [0]
            gi = [0]
            r_hThc = [P.res("hTh_c%d" % i) for i in range(4)]

            def load_hTh(half_, c_):
                t0_ = half_ * TH + c_ * 512
                P.dma(hTh[:, :, c_ * 512:(c_ + 1) * 512], hT_d[:, t0_:t0_ + 512].rearrange("(kc p) n -> p kc n", p=128),
                      reads=[r_hT], writes=[r_hThc[c_]])

            sched = []
            for half in range(2):
                groups = list(range(11)) if (l == 0 or half == 0) else list(range(6))
                sched += [(half, g) for g in groups]

            def load_W(idx_):
                g_ = sched[idx_][1]
                P.dma(W[idx_ % 2], win_d[l][:, g_ * 512:(g_ + 1) * 512].rearrange("(kc p) n -> p kc n", p=128),
                      writes=[r_W[idx_ % 2]], eng="pool")

            load_W(0)
            for c_ in range(4):
                load_hTh(0, c_)
            for idx, (half, g) in enumerate(sched):
                if True:
                    if idx + 1 < len(sched):
                        load_W(idx + 1)
                    wk = idx % 2
                    last_of_half = idx + 1 < len(sched) and sched[idx + 1][0] != half
                    Wg = W[wk]
                    rW = r_W[wk]
                    for tt_ in range(4):
                        r_hTh = r_hThc[tt_]
                        tok0 = half * TH + tt_ * 512
                        if g < 4:
                            if l == 1 and half == 1:
                                chunks = [2, 3]
                            else:
                                chunks = [0, 1, 2, 3]
                            ck = ci[0] % 2
                            ci[0] += 1
                            P.dma(cst[ck], cs_d[:, :, tok0:tok0 + 512], writes=[r_cst[ck]])
                            for c in chunks:
                                for kc in range(8):
                                    mm(psb(c), Wg[:, kc, c * 128:(c + 1) * 128], hTh[:, kc, tt_ * 512:(tt_ + 1) * 512],
                                       kc == 0, kc == 7, [rW, r_hTh], [r_ps[c]])
                            for (c0, dst, r_dst) in ((0, qT, r_qT), (2, kT, r_kT)):
                                if c0 not in chunks:
                                    continue
                                k2 = (c0 // 2)
                                stt("dve", t1[k2], psb(c0), cp(l, g * 4 + c0), cst[ck][:, 0, :], ALU.add, ALU.mult,
                                    [r_ps[c0], r_colp, r_cst[ck]], [r_t1[k2]])
                                stt("dve", t2[k2], psb(c0 + 1), cp(l, g * 4 + c0 + 1), cst[ck][:, 1, :], ALU.add, ALU.mult,
                                    [r_ps[c0 + 1], r_colp, r_cst[ck]], [r_t2[k2]])
                                tt("pool", dst[:, g, tok0:tok0 + 512], t1[k2], t2[k2], ALU.add,
                                   [r_t1[k2], r_t2[k2]], [r_dst])
                        elif g == 4:
                            for j in range(4):
                                blk = tok0 // 128 + j
                                bank = 4 + (j % 2)
                                for kc in range(8):
                                    mm(psb(bank), hTh[:, kc, tt_ * 512 + j * 128:tt_ * 512 + (j + 1) * 128], Wg[:, kc, :],
                                       kc == 0, kc == 7, [rW, r_hTh], [r_ps[bank]])
                                tt("dve", vsb[:, blk, :, 0:128], psb(bank).rearrange("p (h e) -> p h e", e=128), bv_bc, ALU.add,
                                   [r_ps[bank], r_bcl], [r_v])
                        elif g == 5:
                            for c in range(4):
                                bank = 4 + (c % 2)
                                for kc in range(8):
                                    mm(psb(bank), Wg[:, kc, c * 128:(c + 1) * 128], hTh[:, kc, tt_ * 512:(tt_ + 1) * 512],
                                       kc == 0, kc == 7, [rW, r_hTh], [r_ps[bank]])
                                xk = xi[0] % 2
                                xi[0] += 1
                                actf(xs[xk], psb(bank), AF.Identity, [r_ps[bank], r_colp], [r_xs[xk]], bias=cp(l, 20 + c))
                                P.dma(xr_d[c * 128:(c + 1) * 128, tok0:tok0 + 512], xs[xk], reads=[r_xs[xk]], writes=[r_xr])
                        else:
                            func = AF.Gelu if g == 6 else AF.Sigmoid
                            gk = gi[0] % 2
                            gi[0] += 1
                            for c in range(4):
                                bank = 4 + (c % 2)
                                for kc in range(8):
                                    mm(psb(bank), Wg[:, kc, c * 128:(c + 1) * 128], hTh[:, kc, tt_ * 512:(tt_ + 1) * 512],
                                       kc == 0, kc == 7, [rW, r_hTh], [r_ps[bank]])
                                actf(gs[gk][:, c, :], psb(bank), func, [r_ps[bank], r_colp], [r_gs[gk]], bias=cp(l, g * 4 + c))
                            row0 = (g - 6) * 512
                            P.dma(gates_d[row0:row0 + 512, tok0:tok0 + 512].rearrange("(c p) n -> p c n", p=128), gs[gk],
                                  reads=[r_gs[gk]], writes=[r_gates])
                        if last_of_half:
                            load_hTh(half + 1, tt_)
            P.barrier()
            A.release(m0)

        def stage3(l):
            m0 = A.mark()
            wbd = A.bf16(16 * 128).rearrange("p (a b) -> p a b", b=128)
            r_wbd = P.res("wbd")
            P.dma(wbd, rgbd_d[l].rearrange("m p n -> p m n"), writes=[r_wbd], eng="pool")
            cneg = A.f32(8)
            r_cneg = P.res("cneg")
            lamc = colp[:, l * NCOLP + 84:l * NCOLP + 92]
            actf(cneg, lamc, AF.Exp, [r_colp], [r_cneg], scale=-1.0)
            actf(cneg, cneg, AF.Ln, [r_cneg], [r_cneg], bias=1.0)
            ts("dve", cneg, cneg, -8.0, None, ALU.mult, None, [r_cneg], [r_cneg])
            chalf = A.f32(8)
            ts("dve", chalf, cneg, 0.5, None, ALU.mult, None, [r_cneg], [r_cneg])
            hbias = A.f32(16)
            ts("dve", hbias, colp[:, l * NCOLP + 68:l * NCOLP + 84], 0.5, None, ALU.mult, None, [r_colp], [r_cneg])
            xrt = A.f32(T + 4)
            r_xrt = [P.res("xrtA"), P.res("xrtB")]
            xc = A.f32(T)
            r_xc = [P.res("xcA"), P.res("xcB")]
            xcb = A.bf16(T)
            r_xcb = P.res("xcb")
            hf = A.f32(T)
            r_hf = P.res("hf")
            TW = 1024
            hbt = [A.f32(TW) for _ in range(2)]
            r_hbt = [P.res("hbt%d" % i) for i in range(2)]
            NB = 2
            rr = [A.f32(TW) for _ in range(NB)]
            aa = [A.f32(TW) for _ in range(NB)]
            ii = rr
            gg = [A.f32(TW) for _ in range(NB)]
            sq = [A.f32(TW) for _ in range(NB)]
            r_rr = [P.res("rr%d" % i) for i in range(NB)]
            r_aa = [P.res("aa%d" % i) for i in range(NB)]
            r_ii = r_rr
            r_gg = [P.res("gg%d" % i) for i in range(NB)]
            r_sq = [P.res("sq%d" % i) for i in range(NB)]
            ybs = [A.bf16(TW) for _ in range(2)]
            r_ybs = [P.res("ybs%d" % i) for i in range(2)]
            NT = T // TW
            cnt = 0
            memset("pool", xrt[:, 0:2], 0.0, [r_xrt[0]])
            memset("pool", xrt[:, T + 2:T + 4], 0.0, [r_xrt[1]])
            P.dma(xrt[:, 2:T + 2], xr_d[0:128, :], reads=[r_xr], writes=r_xrt)
            for c in range(4):
                for hh, eng in ((0, "dve"), (1, "dve")):
                    o0 = hh * TH
                    dst = xc[:, o0:o0 + TH]
                    ts(eng, dst, xrt[:, o0:o0 + TH], cp(l, 44 + c), cp(l, 64 + c), ALU.mult, ALU.add,
                       r_xrt + [r_colp], [r_xc[hh]])
                    for j in range(1, 5):
                        stt(eng, dst, xrt[:, o0 + j:o0 + j + TH], cp(l, 44 + j * 4 + c), dst, ALU.mult, ALU.add,
                            r_xrt + [r_colp, r_xc[hh]], [r_xc[hh]])
                actf(xcb, xc, AF.Copy, r_xc, [r_xcb])
                if c + 1 < 4:
                    P.dma(xrt[:, 2:T + 2], xr_d[(c + 1) * 128:(c + 2) * 128, :], reads=[r_xr], writes=r_xrt)
                its = [(0, ti, tl) for ti, tl in enumerate(range(NT))] + [(1, ti, tl) for ti, tl in enumerate(range(NT - 1, -1, -1))]

                def ph1(i):
                    d, ti, tl = its[i]
                    k = i % 2
                    sl = slice(tl * TW, (tl + 1) * TW)
                    b0 = 4 * (i % 2)
                    for h2 in range(2):
                        s2 = slice(tl * TW + h2 * 512, tl * TW + (h2 + 1) * 512)
                        mm(psb(b0 + h2), wbd[:, d * 8 + 0 * 4 + c, :], xcb[:, s2], True, True, [r_wbd, r_xcb], [r_ps[b0 + h2]])
                        mm(psb(b0 + 2 + h2), wbd[:, d * 8 + 1 * 4 + c, :], xcb[:, s2], True, True, [r_wbd, r_xcb], [r_ps[b0 + 2 + h2]])
                    ch_ = chalf[:, d * 4 + c:d * 4 + c + 1]
                    rr3 = rr[k].rearrange("p (a b) -> p a b", b=512)
                    actf(rr3, ps[:, b0:b0 + 2, :], AF.Tanh, [r_ps[b0], r_ps[b0 + 1], r_cneg], [r_rr[k]],
                         bias=hbias[:, d * 4 + c:d * 4 + c + 1], scale=0.5)
                    actf(aa[k], rr[k], AF.Exp, [r_rr[k], r_cneg], [r_aa[k]], scale=ch_, bias=ch_)
                    tt("dve", sq[k], aa[k], aa[k], ALU.mult, [r_aa[k]], [r_sq[k]])
                    actf(rr3, ps[:, b0 + 2:b0 + 4, :], AF.Tanh, [r_ps[b0 + 2], r_ps[b0 + 3], r_cneg], [r_ii[k]],
                         bias=hbias[:, 8 + d * 4 + c:9 + d * 4 + c], scale=0.5)
                    ts("dve", sq[k], sq[k], 1.0, -1.0, ALU.min, ALU.mult, [r_sq[k]], [r_sq[k]])
                    stt("dve", gg[k], ii[k], 1.0, xc[:, sl], ALU.add, ALU.mult, [r_ii[k]] + r_xc, [r_gg[k]])

                def ph2(i):
                    d, ti, tl = its[i]
                    k = i % 2
                    sl = slice(tl * TW, (tl + 1) * TW)
                    actf(sq[k], sq[k], AF.Sqrt, [r_sq[k]], [r_sq[k]], bias=1.0)
                    stt("dve", gg[k], sq[k], 0.5, gg[k], ALU.mult, ALU.mult, [r_sq[k], r_gg[k]], [r_gg[k]])
                    if d == 0:
                        init = 0.0 if ti == 0 else hf[:, tl * TW - 1:tl * TW]
                        o_ap, a_ap, b_ap = hf[:, sl], aa[k], gg[k]
                        rdx, wrx = [r_aa[k], r_gg[k], r_hf], [r_hf]
                    else:
                        hk = ti % 2
                        init = 0.0 if ti == 0 else hbt[1 - hk][:, 0:1]
                        o_ap, a_ap, b_ap = hbt[hk][:, ::-1], aa[k][:, ::-1], gg[k][:, ::-1]
                        rdx, wrx = [r_aa[k], r_gg[k], r_hbt[1 - hk]], [r_hbt[hk]]
                    P.op("dve", lambda e, o_ap=o_ap, a_ap=a_ap, b_ap=b_ap, init=init:
                         e.tensor_tensor_scan(o_ap, a_ap, b_ap, init, ALU.mult, ALU.add), rdx, wrx)
                    if d == 1:
                        yk = ti % 2
                        tt("dve", ybs[yk], hf[:, sl], hbt[hk], ALU.add, [r_hf, r_hbt[hk]], [r_ybs[yk]])
                        P.dma(yb_d[c * 128:(c + 1) * 128, sl], ybs[yk], reads=[r_ybs[yk]], writes=[r_yb])

                for i in range(len(its)):
                    ph1(i)
                    if i >= 1:
                        ph2(i - 1)
                ph2(len(its) - 1)
            P.barrier()
            A.release(m0)

        def stage2(l, qT, kT, vsb, r_qT, r_kT, r_v, nqt):
            m0 = A.mark()
            bcl, r_bcl = load_bcl(l)
            lambda_init = 0.8 - 0.6 * math.exp(-0.3 * l)
            lamt = A.f32(256)
            r_lam = P.res("lam")
            lams = A.f32(8)
            lq = bcl[:, 5 * 1024 + 512 + 128:5 * 1024 + 512 + 128 + 256]
            tt("dve", lamt[:, 0:64], lq[:, 0:64], lq[:, 64:128], ALU.mult, [r_bcl], [r_lam])
            tt("dve", lamt[:, 64:128], lq[:, 128:192], lq[:, 192:256], ALU.mult, [r_bcl, r_lam], [r_lam])
            P.op("dve", lambda e: e.reduce_sum(lams[:, 0:2], lamt[:, 0:128].rearrange("p (a b) -> p a b", b=64), axis=AX.X),
                 [r_lam], [r_lam])
            actf(lams[:, 2:4], lams[:, 0:2], AF.Exp, [r_lam], [r_lam])
            tt("dve", lams[:, 4:5], lams[:, 3:4], lams[:, 2:3], ALU.subtract, [r_lam], [r_lam])
            ts("dve", lams[:, 5:6], lams[:, 4:5], -lambda_init, None, ALU.add, None, [r_lam], [r_lam])
            neg_lam = lams[:, 5:6]
            g_bc = bcl[:, 5 * 1024 + 512:5 * 1024 + 512 + 128]
            PT = [A.bf16(2 * 512).rearrange("p (a b) -> p a b", b=512) for _ in range(3)]
            r_PT = [P.res("PT%d" % i) for i in range(3)]
            otok = A.bf16(4 * 512).rearrange("p (a b) -> p a b", b=512)
            r_otok = P.res("otok", multi=True)
            ats = [A.bf16(4 * 512).rearrange("p (a b) -> p a b", b=512) for _ in range(2)]
            r_ats = [P.res("ats%d" % i, multi=True) for i in range(2)]
            o1 = A.f32(128)
            o2 = A.f32(128)
            osq = A.f32(128)
            fin = A.f32(16)
            r_fin = P.res("fin")
            accs = [A.f32(4 * 264).rearrange("p (a b) -> p a b", b=264) for _ in range(2)]
            r_accs = [P.res("accs%d" % i, multi=True) for i in range(2)]

            def acc(m, qb):
                bank = 4 + 2 * m + qb // 2
                off = (qb % 2) * 132
                return ps[:, bank, off:off + 129], bank

            iters = [(qt, h, kc) for qt in range(nqt) for h in range(4) for kc in range(32)]
            NI = len(iters)

            def emit_S(i):
                qt, h, kc = iters[i]
                b0 = 2 * (i % 2)
                for m in range(2):
                    mm(psb(b0 + m), kT[m * 64:(m + 1) * 64, h, kc * 128:(kc + 1) * 128],
                       qT[m * 64:(m + 1) * 64, h, qt * 512:(qt + 1) * 512], True, True,
                       [r_kT, r_qT], [r_ps[b0 + m]])

            def emit_exp(i):
                b0 = 2 * (i % 2)
                pk = i % 3
                actf(PT[pk], ps[:, b0:b0 + 2, :], AF.Exp, [r_ps[b0], r_ps[b0 + 1]], [r_PT[pk]], scale=0.125)

            def emit_PV(i):
                qt, h, kc = iters[i]
                pk = i % 3
                for m in range(2):
                    for qb in range(4):
                        a_ap, bank = acc(m, qb)
                        mm(a_ap, PT[pk][:, m, qb * 128:(qb + 1) * 128], vsb[:, kc, h, 0:129],
                           kc == 0 and qb % 2 == 0, kc == 31, [r_PT[pk], r_v], [r_ps[bank]], sgc=True)

            fin2 = [A.f32(32) for _ in range(2)]
            r_fin2 = [P.res("fin2_%d" % i) for i in range(2)]
            o2b = [A.f32(4 * 128).rearrange("p (a b) -> p a b", b=128) for _ in range(2)]

            def emit_finA(fk):
                ac = accs[fk]
                r_ac = r_accs[fk]
                f = fin2[fk]
                rf = r_fin2[fk]
                for bb in range(4):
                    P.op("dve", lambda e, bb=bb: e.tensor_copy(ac[:, bb, :], ps[:, 4 + bb, 0:264]), [r_ps[4 + bb]], [r_ac])
                P.op("dve", lambda e: e.reciprocal(f[:, 0:4].rearrange("p (a b) -> p a b", b=2), ac[:, 0:2, 128:261:132]), [r_ac], [rf])
                P.op("dve", lambda e: e.reciprocal(f[:, 4:8].rearrange("p (a b) -> p a b", b=2), ac[:, 2:4, 128:261:132]), [r_ac], [rf])
                ts("dve", f[:, 4:8], f[:, 4:8], neg_lam, None, ALU.mult, None, [rf, r_lam], [rf])
                for qb in range(4):
                    off = (qb % 2) * 132
                    a0 = ac[:, qb // 2, off:off + 128]
                    a1 = ac[:, 2 + qb // 2, off:off + 128]
                    ts("dve", o1, a0, f[:, qb:qb + 1], None, ALU.mult, None, [r_ac, rf], [r_fin])
                    stt("dve", o2b[fk][:, qb, :], a1, f[:, 4 + qb:5 + qb], o1, ALU.mult, ALU.add, [r_ac, rf, r_fin], [rf])
                    tt("dve", osq, o2b[fk][:, qb, :], o2b[fk][:, qb, :], ALU.mult, [rf], [r_fin])
                    P.op("dve", lambda e, qb=qb: e.reduce_sum(f[:, 8 + qb:9 + qb], osq, axis=AX.X), [r_fin], [rf])

            def emit_finBC(h, fk):
                f = fin2[fk]
                rf = r_fin2[fk]
                actf(f[:, 12:16], f[:, 8:12], AF.Ln, [rf], [rf], bias=EPS, scale=1.0 / 128.0)
                actf(f[:, 16:20], f[:, 12:16], AF.Exp, [rf], [rf], scale=-0.5)
                ts("dve", f[:, 16:20], f[:, 16:20], 1.0 - lambda_init, None, ALU.mult, None, [rf], [rf])
                for qb in range(4):
                    stt("dve", otok[:, qb, h * 128:(h + 1) * 128], o2b[fk][:, qb, :], f[:, 16 + qb:17 + qb], g_bc, ALU.mult, ALU.mult,
                        [rf, r_bcl], [r_otok])

            def emit_T(qt, half, bank0):
                ak = qt % 2
                for q2 in range(2):
                    qb = 2 * half + q2
                    bank = bank0 + q2
                    pb = psb(bank).rearrange("p (a b) -> p a b", b=128)
                    for c in range(4):
                        mm(pb[:, c, :], otok[:, qb, c * 128:(c + 1) * 128], ident[:], True, True, [r_otok, r_ident], [r_ps[bank]])
                    P.op("dve", lambda e, pb=pb, qb=qb, ak=ak: e.tensor_copy(ats[ak][:, :, qb * 128:(qb + 1) * 128], pb),
                         [r_ps[bank]], [r_ats[ak]])
                if half == 1:
                    P.dma(att_d[:, qt * 512:(qt + 1) * 512].rearrange("(c p) n -> p c n", p=128), ats[ak],
                          reads=[r_ats[ak]], writes=[r_att])

            pending_T = None
            pending_F = None
            fcnt = 0
            emit_S(0)
            emit_S(1)
            for i in range(NI):
                qt, h, kc = iters[i]
                emit_exp(i)
                if pending_T is not None and kc in (12, 13):
                    emit_T(pending_T, kc - 12, 2 * (i % 2))
                    if kc == 13:
                        pending_T = None
                if i + 2 < NI:
                    emit_S(i + 2)
                emit_PV(i)
                if kc == 31:
                    emit_finA(fcnt % 2)
                    pending_F = (qt, h, fcnt % 2)
                    fcnt += 1
                if kc == 6 and pending_F is not None:
                    emit_finBC(pending_F[1], pending_F[2])
                    if pending_F[1] == 3:
                        pending_T = pending_F[0]
                    pending_F = None
            if pending_F is not None:
                emit_finBC(pending_F[1], pending_F[2])
                if pending_F[1] == 3:
                    pending_T = pending_F[0]
            if pending_T is not None:
                emit_T(pending_T, 0, 0)
                emit_T(pending_T, 1, 2)
            P.barrier()
            A.release(m0)

        def stage4(l, bcl, r_bcl, ntiles):
            m0 = A.mark()
            wpa = A.bf16(4 * D).rearrange("p (a b) -> p a b", b=D)
            wpb = A.bf16(4 * D).rearrange("p (a b) -> p a b", b=D)
            wo = A.bf16(8 * D).rearrange("p (a b) -> p a b", b=D)
            r_wpa, r_wpb, r_wo = P.res("wpa"), P.res("wpb"), P.res("wo")
            P.dma(wpa, wpa_d[l].rearrange("(kc p) n -> p kc n", p=128), writes=[r_wpa], eng="pool")
            P.dma(wpb, wpb_d[l].rearrange("(kc p) n -> p kc n", p=128), writes=[r_wpb], eng="pool")
            P.dma(wo, wo_d[l].rearrange("(kc p) n -> p kc n", p=128), writes=[r_wo], eng="pool")
            att_t = [A.bf16(4 * 512).rearrange("p (a b) -> p a b", b=512) for _ in range(2)]
            yb_t = [A.bf16(4 * 512).rearrange("p (a b) -> p a b", b=512) for _ in range(2)]
            g_t = [A.bf16(20 * 512).rearrange("p (a b) -> p a b", b=512) for _ in range(2)]
            h_t1 = A.f32(4 * D).rearrange("p (a b) -> p a b", b=D)
            h_t = [h_t1, h_t1]
            r_htb = [P.res("s4h%d" % i) for i in range(4)]
            r_in = [P.res("s4in%d" % i, multi=True) for i in range(2)]
            ybg = A.bf16(4 * 512).rearrange("p (a b) -> p a b", b=512)
            r_ybg = P.res("ybg")
            merged = A.bf16(8 * 512).rearrange("p (a b) -> p a b", b=512)
            r_merged = P.res("merged", multi=True)
            tA = [A.f32(512) for _ in range(2)]
            tB = [A.f32(512) for _ in range(2)]
            r_tA = [P.res("tA%d" % i) for i in range(2)]
            r_tB = [P.res("tB%d" % i) for i in range(2)]
            x1 = [A.f32(D) for _ in range(2)]
            r_x1 = [P.res("x1_%d" % i) for i in range(2)]
            tmp = [A.f32(D) for _ in range(2)]
            r_tmp = [P.res("tmp%d" % i) for i in range(2)]
            h1 = [A.f32(D) for _ in range(2)]
            r_h1 = [P.res("h1_%d" % i) for i in range(2)]
            xbf = [A.bf16(D) for _ in range(2)]
            r_xbf = [P.res("xbf%d" % i) for i in range(2)]
            hTs = [A.bf16(8 * 512).rearrange("p (a b) -> p a b", b=512) for _ in range(2)]
            r_hTs = [P.res("hTs%d" % i, multi=True) for i in range(2)]
            g1 = bcl[:, 0:D]
            b1 = bcl[:, D:2 * D]
            bo = bcl[:, 4 * D:5 * D]
            EPS2 = EPS / (ALPHA * ALPHA)
            bos = A.f32(D)
            r_bos = P.res("bos")
            ts("dve", bos, bo, 1.0 / ALPHA, None, ALU.mult, None, [r_bcl], [r_bos])
            bi = 0
            pipe = LNPipe()

            def finish(pd):
                (tok0_, blk_, b2_, k_) = pd
                ln_back(h1[b2_], r_h1[b2_], b1, r_bcl, tmp[b2_], r_tmp[b2_])
                emit_h(tok0_ + blk_ * 128, h1[b2_], r_h1[b2_], xbf[b2_], r_xbf[b2_], hTs[k_], r_hTs[k_], blk_, 4,
                       (2 * b2_, 2 * b2_ + 1))

            def load_in(tl_):
                k_ = tl_ % 2
                t0_ = tl_ * 512
                P.dma(att_t[k_], att_d[:, t0_:t0_ + 512].rearrange("(c p) n -> p c n", p=128), reads=[r_att], writes=[r_in[k_]])
                P.dma(yb_t[k_], yb_d[:, t0_:t0_ + 512].rearrange("(c p) n -> p c n", p=128), reads=[r_yb], writes=[r_in[k_]])
                P.dma(g_t[k_], gates_d[:, t0_:t0_ + 512].rearrange("(c p) n -> p c n", p=128), reads=[r_gates], writes=[r_in[k_]])

            load_in(0)
            for tl in range(ntiles):
                k = tl % 2
                tok0 = tl * 512
                if tl + 1 < ntiles:
                    load_in(tl + 1)
                for blk in range(4):
                    P.dma(h_t[k][:, blk, :], hres_d[tok0 + blk * 128:tok0 + (blk + 1) * 128, :], reads=[r_hres], writes=[r_htb[blk]])
                tt("pool", ybg, yb_t[k], g_t[k][:, 0:4, :], ALU.mult, [r_in[k]], [r_ybg])
                for blk in range(4):
                    tt("pool", h_t[k][:, blk, :], h_t[k][:, blk, :], bos, ALU.add, [r_htb[blk], r_bos], [r_htb[blk]])
                for fc in range(8):
                    f2 = fc % 2
                    bA, bB = 2 * f2, 2 * f2 + 1
                    for kc in range(4):
                        mm(psb(bA), wpa[:, kc, fc * 128:(fc + 1) * 128], att_t[k][:, kc, :], kc == 0, kc == 3,
                           [r_wpa, r_in[k]], [r_ps[bA]])
                    for kc in range(4):
                        mm(psb(bB), wpb[:, kc, fc * 128:(fc + 1) * 128], ybg[:, kc, :], kc == 0, kc == 3,
                           [r_wpb, r_ybg], [r_ps[bB]])
                    tt("dve", tA[f2], psb(bA), g_t[k][:, 4 + fc, :], ALU.mult, [r_ps[bA], r_in[k]], [r_tA[f2]])
                    tt("dve", tB[f2], psb(bB), g_t[k][:, 12 + fc, :], ALU.mult, [r_ps[bB], r_in[k]], [r_tB[f2]])
                    tt("dve", merged[:, fc, :], tA[f2], tB[f2], ALU.add, [r_tA[f2], r_tB[f2]], [r_merged])
                for blk in range(4):
                    b2 = bi % 2
                    bi += 1
                    for hh in range(2):
                        bank = 2 * b2 + hh
                        for kc in range(8):
                            mm(psb(bank), merged[:, kc, blk * 128:(blk + 1) * 128], wo[:, kc, hh * 512:(hh + 1) * 512],
                               kc == 0, kc == 7, [r_merged, r_wo], [r_ps[bank]])
                        stt("dve", x1[b2][:, hh * 512:(hh + 1) * 512], psb(bank), 1.0 / ALPHA, h_t[k][:, blk, hh * 512:(hh + 1) * 512],
                            ALU.mult, ALU.add, [r_htb[blk], r_ps[bank]], [r_x1[b2]])
                    pipe.push(dict(src=x1[b2], r_src=r_x1[b2], dst=h1[b2], r_dst=r_h1[b2], g=g1, b=b1, r_bc=r_bcl,
                                   tmp=tmp[b2], r_tmp=r_tmp[b2], tok0=tok0 + blk * 128, final=False, xbf=xbf[b2], r_xbf=r_xbf[b2],
                                   hTs=hTs[k], r_hTs=r_hTs[k], j=blk, pbank=(4 + 2 * b2, 5 + 2 * b2), eps=EPS2))
            pipe.flush()
            P.barrier()
            A.release(m0)

        def stage5(l, bcl, r_bcl, tok_base, final):
            m0 = A.mark()
            moe = (l % 2 == 1)
            hTh = A.bf16(8 * TH).rearrange("p (a b) -> p a b", b=TH)
            r_hThc = [P.res("hTh5_%d" % i) for i in range(4)]
            for c_ in range(4):
                t0_ = tok_base + c_ * 512
                P.dma(hTh[:, :, c_ * 512:(c_ + 1) * 512], hT_d[:, t0_:t0_ + 512].rearrange("(kc p) n -> p kc n", p=128),
                      reads=[r_hT], writes=[r_hThc[c_]])
            yacc = A.f32(16 * D).rearrange("p (a b) -> p a b", b=D)
            r_y = [P.res("yacc%d" % i) for i in range(16)]
            for q4 in range(4):
                P.dma(yacc[:, q4 * 4:(q4 + 1) * 4, :],
                      hres_d[tok_base + q4 * 512:tok_base + (q4 + 1) * 512, :].rearrange("(b p) d -> p b d", p=128),
                      reads=[r_hres], writes=r_y[q4 * 4:(q4 + 1) * 4])
            comb = None
            r_comb = P.res("comb", multi=True)
            if moe:
                j = l // 2
                wr = A.bf16(8 * NE).rearrange("p (a b) -> p a b", b=NE)
                r_wr = P.res("wr")
                P.dma(wr, mwr_d[j].rearrange("(kc p) n -> p kc n", p=128), writes=[r_wr], eng="pool")
                comb = A.f32(16 * NE).rearrange("p (a b) -> p a b", b=NE)
                lg = A.f32(NE)
                m8 = A.f32(8)
                ex = A.f32(NE)
                msk = A.f32(NE)
                sc = A.f32(8)
                r_rt = P.res("router")
                br_bc = bcl[:, 5 * 1024 + 512 + 128 + 256:5 * 1024 + 512 + 128 + 256 + NE]
                for b in range(16):
                    bank = b % 2
                    for kc in range(8):
                        mm(ps[:, bank, 0:NE], hTh[:, kc, b * 128:(b + 1) * 128], wr[:, kc, :], kc == 0, kc == 7,
                           [r_hThc[b // 4], r_wr], [r_ps[bank]])
                    tt("dve", lg, ps[:, bank, 0:NE], br_bc, ALU.add, [r_ps[bank], r_bcl], [r_rt])
                    P.op("dve", lambda e: e.max(out=m8, in_=lg), [r_rt], [r_rt])
                    ts("dve", sc[:, 0:1], m8[:, 0:1], -1.0, None, ALU.mult, None, [r_rt], [r_rt])
                    ts("dve", msk, lg, m8[:, 1:2], None, ALU.is_ge, None, [r_rt], [r_rt])
                    actf(ex, lg, AF.Exp, [r_rt], [r_rt], bias=sc[:, 0:1])
                    tt("dve", ex, ex, msk, ALU.mult, [r_rt], [r_rt])
                    P.op("dve", lambda e: e.reduce_sum(sc[:, 1:2], ex, axis=AX.X), [r_rt], [r_rt])
                    P.op("dve", lambda e: e.reciprocal(sc[:, 2:3], sc[:, 1:2]), [r_rt], [r_rt])
                    ts("dve", comb[:, b, :], ex, sc[:, 2:3], 1.0 / ALPHA, ALU.mult, ALU.mult, [r_rt], [r_comb])
                experts = [(mwg_d[j][e], mwu_d[j][e], mwd_d[j][e], e, DFE) for e in range(NE)]
            else:
                j = l // 2
                experts = [(fwg_d[j], fwu_d[j], fwd_d[j], None, DFF)]
            wg = [A.bf16(8 * 512).rearrange("p (a b) -> p a b", b=512) for _ in range(2)]
            wu = [A.bf16(8 * 512).rearrange("p (a b) -> p a b", b=512) for _ in range(2)]
            wd = [A.bf16(4 * D).rearrange("p (a b) -> p a b", b=D) for _ in range(2)]
            r_wk = [P.res("w5_%d" % i, multi=True) for i in range(2)]
            aT = [A.bf16(4 * 512).rearrange("p (a b) -> p a b", b=512) for _ in range(2)]
            r_aT = [P.res("aT%d" % i, multi=True) for i in range(2)]
            sg = [A.f32(512) for _ in range(2)]
            r_sg = [P.res("sg%d" % i) for i in range(2)]
            g2 = bcl[:, 2 * D:3 * D]
            b2_ = bcl[:, 3 * D:4 * D]
            tmp = [A.f32(D) for _ in range(2)]
            r_tmp = [P.res("tmp5_%d" % i) for i in range(2)]
            h2 = [A.f32(D) for _ in range(2)]
            r_h2 = [P.res("h2_%d" % i) for i in range(2)]
            if not final:
                xbf1 = A.bf16(D)
                xbf = [xbf1, xbf1]
                r_xbf1 = P.res("xbf5")
                r_xbf = [r_xbf1, r_xbf1]
                hTs1 = A.bf16(8 * 512).rearrange("p (a b) -> p a b", b=512)
                hTs = [hTs1, hTs1]
                r_hTs1 = P.res("hTs5", multi=True)
                r_hTs = [r_hTs1, r_hTs1]
            pipe = LNPipe()

            def push_block(b):
                k = b % 2
                job = dict(src=yacc[:, b, :], r_src=r_y[b], dst=h2[k], r_dst=r_h2[k], g=g2, b=b2_, r_bc=r_bcl,
                           tmp=tmp[k], r_tmp=r_tmp[k], tok0=tok_base + b * 128, final=final, eps=EPS / (ALPHA * ALPHA))
                if not final:
                    sg_ = (b // 4) % 2
                    job.update(xbf=xbf[k], r_xbf=r_xbf[k], hTs=hTs[sg_], r_hTs=r_hTs[sg_], j=b % 4, pbank=(2 * k, 2 * k + 1))
                pipe.push(job)
            groups5 = []
            for (wg_ap, wu_ap, wd_ap, eidx, F) in experts:
                nch = F // 128
                for c0 in range(0, nch, 4):
                    groups5.append((wg_ap, wu_ap, wd_ap, eidx, c0, min(4, nch - c0)))
            units5 = [(gi_, t4) for gi_ in range(len(groups5)) for t4 in range(4)]
            loaded5 = set()
            cnt5 = {"si": 0, "di": 0}

            def ensure_w(gi_):
                if gi_ in loaded5:
                    return
                loaded5.add(gi_)
                (wg_ap, wu_ap, wd_ap, eidx, c0, ncg) = groups5[gi_]
                k = gi_ % 2
                P.dma(wg[k][:, :, 0:ncg * 128], wg_ap[:, c0 * 128:(c0 + ncg) * 128].rearrange("(kc p) n -> p kc n", p=128),
                      writes=[r_wk[k]], eng="pool")
                P.dma(wu[k][:, :, 0:ncg * 128], wu_ap[:, c0 * 128:(c0 + ncg) * 128].rearrange("(kc p) n -> p kc n", p=128),
                      writes=[r_wk[k]], eng="pool")
                P.dma(wd[k][:, 0:ncg, :], wd_ap[c0 * 128:(c0 + ncg) * 128, :].rearrange("(c p) n -> p c n", p=128),
                      writes=[r_wk[k]], eng="pool")

            def emit_GU(u, c):
                gi_, t4 = units5[u]
                ncg = groups5[gi_][5]
                if c >= ncg:
                    return
                ensure_w(gi_)
                k = gi_ % 2
                a2 = u % 2
                s2 = cnt5["si"] % 2
                cnt5["si"] += 1
                bG, bU = 2 * s2, 2 * s2 + 1
                for kc in range(8):
                    mm(psb(bG), wg[k][:, kc, c * 128:(c + 1) * 128], hTh[:, kc, t4 * 512:(t4 + 1) * 512],
                       kc == 0, kc == 7, [r_wk[k], r_hThc[t4]], [r_ps[bG]])
                for kc in range(8):
                    mm(psb(bU), wu[k][:, kc, c * 128:(c + 1) * 128], hTh[:, kc, t4 * 512:(t4 + 1) * 512],
                       kc == 0, kc == 7, [r_wk[k], r_hThc[t4]], [r_ps[bU]])
                actf(sg[s2], psb(bG), AF.Silu, [r_ps[bG]], [r_sg[s2]])
                tt("dve", aT[a2][:, c, :], sg[s2], psb(bU), ALU.mult, [r_sg[s2], r_ps[bU]], [r_aT[a2]])

            def emit_D(u):
                gi_, t4 = units5[u]
                eidx, ncg = groups5[gi_][3], groups5[gi_][5]
                k = gi_ % 2
                a2 = u % 2
                for blk in range(4):
                    b = t4 * 4 + blk
                    for hh in range(2):
                        bank = 4 + 2 * (cnt5["di"] % 2) + hh
                        for c in range(ncg):
                            mm(psb(bank), aT[a2][:, c, blk * 128:(blk + 1) * 128], wd[k][:, c, hh * 512:(hh + 1) * 512],
                               c == 0, c == ncg - 1, [r_aT[a2], r_wk[k]], [r_ps[bank]])
                        sc_ = (1.0 / ALPHA) if eidx is None else comb[:, b, eidx:eidx + 1]
                        rd = [r_ps[bank], r_y[b]] + ([] if eidx is None else [r_comb])
                        stt("dve", yacc[:, b, hh * 512:(hh + 1) * 512], psb(bank), sc_, yacc[:, b, hh * 512:(hh + 1) * 512],
                            ALU.mult, ALU.add, rd, [r_y[b]])
                    cnt5["di"] += 1

            emit_GU(0, 0)
            for u in range(len(units5)):
                for c in range(1, 4):
                    emit_GU(u, c)
                if u + 1 < len(units5):
                    emit_GU(u + 1, 0)
                emit_D(u)
            for b in range(16):
                push_block(b)
            pipe.flush()
            P.barrier()
            A.release(m0)

        stage0()
        for l in range(L):
            if upto < 1 + 10 * l:
                break
            ml = A.mark()
            qT = A.bf16(4 * T).rearrange("p (a b) -> p a b", b=T)
            kT = A.bf16(4 * T).rearrange("p (a b) -> p a b", b=T)
            vsb = A.bf16(32 * 4 * 130).rearrange("p (a b c) -> p a b c", b=4, c=130)
            r_qT, r_kT, r_v = P.res("qT", multi=True), P.res("kT", multi=True), P.res("v", multi=True)
            stage1(l, qT, kT, vsb, r_qT, r_kT, r_v)
            if debug and upto == 1 + 10 * l:
                P.dma(dbg["qT"], qT.rearrange("p a b -> p (a b)"), reads=[r_qT])
                P.dma(dbg["kT"], kT.rearrange("p a b -> p (a b)"), reads=[r_kT])
                P.dma(dbg["v"], vsb.rearrange("p a b c -> p (a b c)"), reads=[r_v])
            if upto < 2 + 10 * l:
                break
            stage3(l)
            if upto < 3 + 10 * l:
                break
            stage2(l, qT, kT, vsb, r_qT, r_kT, r_v, 8 if l == 0 else 4)
            A.release(ml)
            bcl, r_bcl = load_bcl(l)
            if upto < 4 + 10 * l:
                break
            stage4(l, bcl, r_bcl, 8 if l == 0 else 4)
            if upto < 5 + 10 * l:
                break
            if l == 0:
                stage5(l, bcl, r_bcl, 0, False)
                stage5(l, bcl, r_bcl, TH, False)
            else:
                stage5(l, bcl, r_bcl, 0, True)
            P.barrier()
            A.release(ml)

        P.finalize(st)
        with nc.Block() as block:
            P.emit(block)
    return nc


def _rope_tables():
    half = 32
    inv = (1.0 / (10000.0 ** (np.arange(half, dtype=np.float32) * np.float32(2.0 / 64)))).astype(np.float32)
    pos = np.arange(T, dtype=np.float32)
    ang = pos[:, None] * inv[None, :]
    cos = np.cos(ang).astype(np.float32)
    sin = np.sin(ang).astype(np.float32)
    cs = np.zeros((128, 2, T), np.float32)
    for p in range(128):
        d = p % 64
        i = d % 32
        cs[p, 0] = cos[:, i]
        cs[p, 1] = -sin[:, i] if d < 32 else sin[:, i]
    return cs


def _prep_inputs(inp):
    f = lambda a: np.ascontiguousarray(np.asarray(a, dtype=np.float32))
    w_in = f(inp["w_in"])
    b_in = f(inp["b_in"])
    idx = np.arange(1024)
    blk = idx // 64
    d = idx % 64
    swap_idx = blk * 64 + (d + 32) % 64
    ext_cols = []
    for h in range(4):
        qh = np.arange(h * 128, (h + 1) * 128)
        kh = 512 + qh
        ext_cols += [qh, swap_idx[qh], kh, swap_idx[kh]]
    ext_cols.append(np.arange(1024, 4608))
    ext_cols = np.concatenate(ext_cols)
    w_in_ext = np.ascontiguousarray(w_in[:, :, ext_cols])
    b_ext = b_in[:, ext_cols]

    cs = _rope_tables()
    ident = np.eye(128, dtype=np.float32)
    bc0 = np.ascontiguousarray(np.broadcast_to(
        np.concatenate([f(inp["ln_in_g"]), f(inp["ln_in_b"])])[None, :], (128, 2 * D)))

    def bcl_for():
        rows = []
        for l in range(L):
            lam4 = np.concatenate([f(inp["lam_q1"])[l], f(inp["lam_k1"])[l], f(inp["lam_q2"])[l], f(inp["lam_k2"])[l]])
            row = np.concatenate([f(inp["ln1_g"])[l], f(inp["ln1_b"])[l], f(inp["ln2_g"])[l], f(inp["ln2_b"])[l],
                                  f(inp["b_o"])[l], b_in[l, 1024:1536], f(inp["subln_g"])[l], lam4,
                                  f(inp["moe_br"])[0]])
            assert row.shape[0] == BCL
            rows.append(np.broadcast_to(row[None, :], (128, BCL)))
        return np.ascontiguousarray(np.stack(rows))
    bcl = bcl_for()

    conv_w = f(inp["conv_w"])
    conv_b = f(inp["conv_b"])
    rg_wa, rg_wx = f(inp["rg_wa"]), f(inp["rg_wx"])
    rg_ba, rg_bx, rg_lam = f(inp["rg_ba"]), f(inp["rg_bx"]), f(inp["rg_lam"])

    def per_parity(rev):
        colp = np.zeros((128, L * NCOLP), np.float32)
        rgbd = np.zeros((L, 16, 128, 128), np.float32)
        for l in range(L):
            base = l * NCOLP
            colp[:, base:base + 44] = b_ext[l].reshape(44, 128).T
            w5 = np.zeros((5, 512), np.float32)
            if not rev:
                w5[0:4] = conv_w[l]
            else:
                w5[1:5] = conv_w[l][::-1]
            for j in range(5):
                colp[:, base + 44 + j * 4:base + 44 + (j + 1) * 4] = w5[j].reshape(4, 128).T
            colp[:, base + 64:base + 68] = conv_b[l].reshape(4, 128).T
            for dd in range(2):
                src = (1 - dd) if rev else dd
                colp[:, base + 68 + dd * 4:base + 72 + dd * 4] = rg_ba[l, src].reshape(4, 128).T
                colp[:, base + 76 + dd * 4:base + 80 + dd * 4] = rg_bx[l, src].reshape(4, 128).T
                colp[:, base + 84 + dd * 4:base + 88 + dd * 4] = rg_lam[l, src].reshape(4, 128).T
                for ax, wsrc in enumerate((rg_wa, rg_wx)):
                    for c in range(4):
                        mtx = rgbd[l, dd * 8 + ax * 4 + c]
                        mtx[0:64, 0:64] = wsrc[l, src, 2 * c]
                        mtx[64:128, 64:128] = wsrc[l, src, 2 * c + 1]
        return colp, rgbd

    par = [per_parity(False), per_parity(True)]
    cs_rev = np.ascontiguousarray(cs[:, :, ::-1])
    x = f(inp["x"])
    shared = {
        "bc0": bc0, "bcl": bcl, "ident": ident, "w_in_ext": w_in_ext,
        "w_pa": f(inp["w_pa"]), "w_pb": f(inp["w_pb"]), "w_o": f(inp["w_o"]),
        "ffn_wg": f(inp["ffn_wg"]), "ffn_wu": f(inp["ffn_wu"]), "ffn_wd": f(inp["ffn_wd"]),
        "moe_wr": f(inp["moe_wr"]), "moe_wg": f(inp["moe_wg"]), "moe_wu": f(inp["moe_wu"]),
        "moe_wd": f(inp["moe_wd"]),
    }
    in_maps = []
    for c in range(8):
        b, rev = c // 2, c % 2
        m = dict(shared)
        m["x"] = np.ascontiguousarray(x[b, ::-1]) if rev else np.ascontiguousarray(x[b])
        m["cs"] = cs_rev if rev else cs
        m["colp"], m["rg_bd"] = par[rev]
        in_maps.append(m)
    return in_maps


_NC_CACHE = {}


def kernel(**inputs):
    in_maps = _prep_inputs(inputs)
    if "nc" not in _NC_CACHE:
        _NC_CACHE["nc"] = build_program()
    nc = _NC_CACHE["nc"]
    res = run_bass_kernel_spmd(nc, in_maps, core_ids=list(range(8)))
    out = np.zeros((4, T, D), np.float32)
    for c in range(8):
        o = np.asarray(res.results[c]["out"], dtype=np.float32)
        b, rev = c // 2, c % 2
        if rev:
            out[b, TH:] = o[::-1]
        else:
            out[b, :TH] = o
    return out
```

```python
import math
from contextlib import ExitStack

import numpy as np
import concourse.bass as bass
import concourse.mybir as mybir
from concourse.bass_utils import run_bass_kernel_spmd

F32 = mybir.dt.float32
BF16 = mybir.dt.bfloat16
AF = mybir.ActivationFunctionType
ALU = mybir.AluOpType
AX = mybir.AxisListType

ENGS = ("pe", "act", "dve", "pool", "sp")
N_DMA_SLOTS = 8

D = 1024
T = 4096
TH = 2048
L = 2
NE = 8
DFF = 2816
DFE = 3584
ALPHA = (2.0 * L) ** 0.25
EPS = 1e-5
NCOLP = 44 + 5 * 4 + 4 + 8 + 8 + 8
BCL = 4 * 1024 + 1024 + 512 + 128 + 256 + 8


class Res:
    __slots__ = ("name", "writers", "readers", "multi", "ignore")

    def __init__(self, name, multi=False, ignore=False):
        self.name = name
        self.writers = []
        self.readers = []
        self.multi = multi
        self.ignore = ignore


class Op:
    __slots__ = ("eng", "fn", "deps", "dma", "slot", "sig", "needed", "idx")


class Prog:
    def __init__(self, nc, same_engine_sync=("act", "dve", "pool")):
        self.nc = nc
        self.ops = []
        self.same = set(same_engine_sync)
        self.dma_count = {"sp": 0, "pool": 0, "act": 0}
        self.last_eng = {}
        self.last_slot = {}

    def res(self, name, multi=False, ignore=False):
        return Res(name, multi, ignore)

    def op(self, eng, fn, reads=(), writes=(), dma=False, extra_deps=()):
        o = Op()
        o.eng, o.fn, o.dma = eng, fn, dma
        o.idx = len(self.ops)
        deps = set(extra_deps)
        reads = [r for r in reads if not r.ignore]
        writes = [w for w in writes if not w.ignore]
        for r in reads:
            deps.update(r.writers)
        for w in writes:
            deps.update(w.readers)
            if not (w.multi and not w.readers):
                deps.update(w.writers)
        for w in writes:
            if w.multi and not w.readers:
                w.writers.append(o.idx)
            else:
                w.writers = [o.idx]
            w.readers = []
        for r in reads:
            if r not in writes:
                r.readers.append(o.idx)
        o.deps = deps
        o.needed = False
        o.slot = None
        o.sig = None
        if dma:
            o.slot = self.dma_count[eng] % N_DMA_SLOTS
            self.dma_count[eng] += 1
            self.last_slot[(eng, o.slot)] = o.idx
        elif fn is not None:
            self.last_eng[eng] = o.idx
        self.ops.append(o)
        return o

    def dma(self, out, in_, reads=(), writes=(), eng="sp"):
        return self.op(eng, lambda e: e.dma_start(out=out, in_=in_), reads, writes, dma=True)

    def barrier(self):
        tails = list(self.last_eng.values()) + list(self.last_slot.values())
        for eng in ENGS:
            self.op(eng, None, extra_deps=tails)

    def finalize(self, stack):
        nc = self.nc
        ops = self.ops
        for o in ops:
            keep = set()
            for d in o.deps:
                p = ops[d]
                if p.fn is None:
                    continue
                if (not p.dma) and p.eng == o.eng and (o.eng not in self.same) and not o.dma:
                    continue
                if (not p.dma) and p.eng == o.eng and o.fn is None:
                    continue
                keep.add(d)
            o.deps = keep
            for d in keep:
                ops[d].needed = True
        esem = {e: stack.enter_context(nc.semaphore("sem_" + e)) for e in ENGS}
        dsem = {}
        for q in ("sp", "pool", "act"):
            if self.dma_count[q]:
                dsem[q] = [stack.enter_context(nc.semaphore("dsem_%s%d" % (q, i)))
                           for i in range(N_DMA_SLOTS)]
        ecount = {e: 0 for e in ENGS}
        dcount = {q: [0] * N_DMA_SLOTS for q in dsem}
        for o in ops:
            if o.dma:
                c = dcount[o.eng]
                c[o.slot] += 1
                o.sig = (dsem[o.eng][o.slot], 16 * c[o.slot], 16)
            elif o.needed:
                ecount[o.eng] += 1
                o.sig = (esem[o.eng], ecount[o.eng], 1)
        self.max_counts = dict(ecount)
        per_eng = {e: [] for e in ENGS}
        seen = {e: {} for e in ENGS}
        for o in ops:
            waits = []
            cand = {}
            for d in o.deps:
                s, v, _ = ops[d].sig
                k = id(s)
                if k not in cand or cand[k][1] < v:
                    cand[k] = (s, v)
            if o.dma:
                s, v, _ = o.sig
                if v > 16:
                    k = id(s)
                    if k not in cand or cand[k][1] < v - 16:
                        cand[k] = (s, v - 16)
            for k, (s, v) in cand.items():
                if seen[o.eng].get(k, 0) >= v:
                    continue
                seen[o.eng][k] = v
                waits.append((s, v))
            per_eng[o.eng].append((o, waits))
        self.per_eng = per_eng
        self.final_sigs = {}
        for o in ops:
            if o.sig is not None:
                self.final_sigs[id(o.sig[0])] = (o.sig[0], o.sig[1])

    def emit(self, block, final_wait_eng="sp"):
        per_eng = self.per_eng
        finals = list(self.final_sigs.values())

        def run(eng_name):
            def body(e):
                for o, waits in per_eng[eng_name]:
                    for s, v in waits:
                        e.wait_ge(s, v)
                    if o.fn is None:
                        continue
                    ins = o.fn(e)
                    if o.sig is not None:
                        ins.then_inc(o.sig[0], o.sig[2])
                if eng_name == final_wait_eng:
                    for s, v in finals:
                        e.wait_ge(s, v)
            return body

        block.tensor(run("pe"))
        block.scalar(run("act"))
        block.vector(run("dve"))
        block.gpsimd(run("pool"))
        block.sync(run("sp"))


class Arena:
    def __init__(self, t, n_f32):
        self.t = t
        self.n = n_f32
        self.off = 0

    def mark(self):
        return self.off

    def release(self, m):
        self.peak = max(getattr(self, "peak", 0), self.off)
        self.off = m

    def f32(self, n):
        n4 = (n + 7) // 8 * 8
        assert self.off + n4 <= self.n, ("arena overflow", self.off, n4, self.n)
        ap = self.t[:, self.off:self.off + n]
        self.off += n4
        return ap

    def bf16(self, n):
        nf = (n + 1) // 2
        nf = (nf + 7) // 8 * 8
        assert self.off + nf <= self.n, ("arena overflow", self.off, nf, self.n)
        ap = self.t[:, self.off:self.off + nf].bitcast(BF16)[:, 0:n]
        self.off += nf
        return ap


def build_program(upto=99, debug=False):
    nc = bass.Bass("TRN2", target_bir_lowering=False)
    okind = "ExternalOutput" if debug else "Internal"

    def din(name, shape, dt=F32):
        return nc.dram_tensor(name, list(shape), dt, kind="ExternalInput").ap()

    x_d = din("x", [T, D])
    bc0_d = din("bc0", [128, 2 * D])
    bcl_d = din("bcl", [L, 128, BCL])
    colp_d = din("colp", [128, L * NCOLP])
    cs_d = din("cs", [128, 2, T])
    ident_d = din("ident", [128, 128])
    win_d = din("w_in_ext", [L, D, 5632])
    rgbd_d = din("rg_bd", [L, 16, 128, 128])
    wpa_d = din("w_pa", [L, 512, D])
    wpb_d = din("w_pb", [L, 512, D])
    wo_d = din("w_o", [L, D, D])
    fwg_d = din("ffn_wg", [1, D, DFF])
    fwu_d = din("ffn_wu", [1, D, DFF])
    fwd_d = din("ffn_wd", [1, DFF, D])
    mwr_d = din("moe_wr", [1, D, NE])
    mwg_d = din("moe_wg", [1, NE, D, DFE])
    mwu_d = din("moe_wu", [1, NE, D, DFE])
    mwd_d = din("moe_wd", [1, NE, DFE, D])

    out_d = nc.dram_tensor("out", [TH, D], F32, kind="ExternalOutput").ap()
    hres_d = nc.dram_tensor("hres", [T, D], F32, kind=okind).ap()
    hT_d = nc.dram_tensor("hT", [D, T], BF16, kind=okind).ap()
    xr_d = nc.dram_tensor("xr", [512, T], F32, kind=okind).ap()
    gates_d = nc.dram_tensor("gates", [2560, T], BF16, kind=okind).ap()
    yb_d = nc.dram_tensor("yb", [512, T], BF16, kind=okind).ap()
    att_d = nc.dram_tensor("att", [512, T], BF16, kind=okind).ap()
    dbg = {}
    if debug:
        dbg["qT"] = nc.dram_tensor("dbg_qT", [128, 4 * T], BF16, kind="ExternalOutput").ap()
        dbg["kT"] = nc.dram_tensor("dbg_kT", [128, 4 * T], BF16, kind="ExternalOutput").ap()
        dbg["v"] = nc.dram_tensor("dbg_v", [128, 32 * 4 * 130], BF16, kind="ExternalOutput").ap()
        dbg["acc"] = nc.dram_tensor("dbg_acc", [128, 4 * 512], F32, kind="ExternalOutput").ap()
        dbg["pt"] = nc.dram_tensor("dbg_pt", [128, 1024], BF16, kind="ExternalOutput").ap()
        dbg["otok"] = nc.dram_tensor("dbg_otok", [128, 2048], BF16, kind="ExternalOutput").ap()

    with ExitStack() as st:
        NAR = 51 * 1024 + 512
        arena_t = st.enter_context(nc.sbuf_tensor("arena", [128, NAR], F32))
        ident = st.enter_context(nc.sbuf_tensor("ident_sb", [128, 128], BF16))
        colp = st.enter_context(nc.sbuf_tensor("colp_sb", [128, L * NCOLP], F32))
        small = st.enter_context(nc.sbuf_tensor("small", [128, 64], F32))
        ps = st.enter_context(nc.psum_tensor("ps", [128, 8, 512], F32))
        A = Arena(arena_t, NAR)
        P = Prog(nc)
        r_ps = [P.res("ps%d" % i) for i in range(8)]
        r_ident = P.res("ident")
        r_colp = P.res("colp")
        r_small = P.res("small")
        r_hres, r_hT, r_xr, r_gates, r_yb, r_att, r_out = [P.res(n, ignore=True) for n in
                                                           "hres hT xr gates yb att out".split()]

        P.dma(ident[:], ident_d, writes=[r_ident], eng="pool")
        P.dma(colp[:], colp_d, writes=[r_colp])

        def psb(i):
            return ps[:, i, :]

        def psb16(i):
            return ps[:, i, :].bitcast(BF16)

        def mm(out, lhsT, rhs, start, stop, reads, writes, sgc=False):
            if sgc:
                P.op("pe", lambda e: e.matmul(out, lhsT, rhs, start=start, stop=stop, skip_group_check=True), reads, writes)
            else:
                P.op("pe", lambda e: e.matmul(out, lhsT, rhs, start=start, stop=stop), reads, writes)

        def actf(out, in_, func, reads, writes, bias=None, scale=None):
            kw = {}
            if bias is not None:
                kw["bias"] = bias
            if scale is not None:
                kw["scale"] = scale
            P.op("act", lambda e: e.activation(out=out, in_=in_, func=func, **kw), reads, writes)

        def tt(eng, out, in0, in1, op, reads, writes):
            P.op(eng, lambda e: e.tensor_tensor(out, in0, in1, op), reads, writes)

        def ts(eng, out, in0, s1, s2, op0, op1, reads, writes):
            if op1 is None:
                P.op(eng, lambda e: e.tensor_scalar(out, in0, s1, None, op0), reads, writes)
            else:
                P.op(eng, lambda e: e.tensor_scalar(out, in0, s1, s2, op0, op1), reads, writes)

        def stt(eng, out, in0, scalar, in1, op0, op1, reads, writes):
            P.op(eng, lambda e: e.scalar_tensor_tensor(out, in0, scalar, in1, op0, op1), reads, writes)

        def memset(eng, ap, val, writes):
            P.op(eng, lambda e: e.memset(ap, val), (), writes)

        def cp(l, i):
            return colp[:, l * NCOLP + i:l * NCOLP + i + 1]

        ln_ctr = [0]

        def layernorm(src, r_src, dst, r_dst, g_bc, b_bc, r_bc, tmp, r_tmp, front_only=False, eps=EPS):
            k = ln_ctr[0] % 2
            ln_ctr[0] += 1
            stats = small[:, k * 16:k * 16 + 12]
            mv = small[:, k * 16 + 12:k * 16 + 14]
            rstd = small[:, k * 16 + 14:k * 16 + 15]
            nmr = small[:, k * 16 + 15:k * 16 + 16]
            rs = r_smallk[k]
            P.op("dve", lambda e: e.bn_stats(stats[:, 0:6], src[:, 0:512]), [r_src], [rs])
            P.op("dve", lambda e: e.bn_stats(stats[:, 6:12], src[:, 512:1024]), [r_src], [rs])
            P.op("dve", lambda e: e.bn_aggr(mv, stats), [rs], [rs])
            actf(rstd, mv[:, 1:2], AF.Sqrt, [rs], [rs], bias=eps)
            P.op("dve", lambda e: e.reciprocal(rstd, rstd), [rs], [rs])
            stt("dve", nmr, mv[:, 0:1], -1.0, rstd, ALU.mult, ALU.mult, [rs], [rs])
            actf(tmp, src, AF.Identity, [r_src, rs], [r_tmp], bias=nmr, scale=rstd)
            tt("dve", tmp, tmp, g_bc, ALU.mult, [r_tmp, r_bc], [r_tmp])
            if not front_only:
                ln_back(dst, r_dst, b_bc, r_bc, tmp, r_tmp)

        def ln_back(dst, r_dst, b_bc, r_bc, tmp, r_tmp):
            tt("dve", dst, tmp, b_bc, ALU.add, [r_tmp, r_bc], [r_dst])

        r_smallk = [P.res("small0"), P.res("small1"), P.res("small2"), P.res("small3")]

        def emit_h(tok0, hblk, r_hblk, xbf, r_xbf, hTs, r_hTs, j, nblk_stage, pbank, write_hres=True):
            if write_hres:
                P.dma(hres_d[tok0:tok0 + 128, :], hblk, reads=[r_hblk], writes=[r_hres])
            actf(xbf, hblk, AF.Copy, [r_hblk], [r_xbf])
            for i2 in range(2):
                bank = pbank[i2]
                pb = psb(bank).rearrange("p (a b) -> p a b", b=128)
                for k4 in range(4):
                    kc = i2 * 4 + k4
                    mm(pb[:, k4, :], xbf[:, kc * 128:(kc + 1) * 128], ident[:], True, True, [r_xbf, r_ident], [r_ps[bank]])
                P.op("dve", lambda e, pb=pb, i2=i2: e.tensor_copy(hTs[:, i2 * 4:(i2 + 1) * 4, j * 128:(j + 1) * 128], pb),
                     [r_ps[bank]], [r_hTs])
            if j == nblk_stage - 1:
                t0 = tok0 - j * 128
                P.dma(hT_d[:, t0:t0 + nblk_stage * 128].rearrange("(kc p) n -> p kc n", p=128), hTs,
                      reads=[r_hTs], writes=[r_hT])

        class LNPipe:
            def __init__(self):
                self.q = []
                self.n = 0

            def push(self, job):
                job["slot"] = self.n % 4
                self.n += 1
                self.q.insert(0, job)
                self._step()

            def _step(self):
                q = self.q
                if len(q) > 4 and q[4] is not None:
                    self.phE(q[4])
                if len(q) > 3 and q[3] is not None:
                    self.phD(q[3])
                if len(q) > 2 and q[2] is not None:
                    self.phC(q[2])
                if len(q) > 1 and q[1] is not None:
                    self.phB(q[1])
                if len(q) > 0 and q[0] is not None:
                    self.phA(q[0])
                if len(q) > 4:
                    q.pop()

            def flush(self):
                for _ in range(4):
                    self.q.insert(0, None)
                    self._step()
                self.q = []

            @staticmethod
            def _sm(job):
                k = job["slot"]
                return (small[:, k * 16:k * 16 + 12], small[:, k * 16 + 12:k * 16 + 14],
                        small[:, k * 16 + 14:k * 16 + 15], small[:, k * 16 + 15:k * 16 + 16], r_smallk[k])

            def phA(self, j):
                stats, mv, rstd, nmr, rs = self._sm(j)
                src, r_src = j["src"], j["r_src"]
                P.op("dve", lambda e: e.bn_stats(stats[:, 0:6], src[:, 0:512]), [r_src], [rs])
                P.op("dve", lambda e: e.bn_stats(stats[:, 6:12], src[:, 512:1024]), [r_src], [rs])
                P.op("dve", lambda e: e.bn_aggr(mv, stats), [rs], [rs])
                actf(rstd, mv[:, 1:2], AF.Sqrt, [rs], [rs], bias=j.get("eps", EPS))

            def phB(self, j):
                stats, mv, rstd, nmr, rs = self._sm(j)
                P.op("dve", lambda e: e.reciprocal(rstd, rstd), [rs], [rs])
                stt("dve", nmr, mv[:, 0:1], -1.0, rstd, ALU.mult, ALU.mult, [rs], [rs])
                actf(j["tmp"], j["src"], AF.Identity, [j["r_src"], rs], [j["r_tmp"]], bias=nmr, scale=rstd)

            def phC(self, j):
                tmp, r_tmp, dst, r_dst = j["tmp"], j["r_tmp"], j["dst"], j["r_dst"]
                tt("dve", tmp, tmp, j["g"], ALU.mult, [r_tmp, j["r_bc"]], [r_tmp])
                tt("dve", dst, tmp, j["b"], ALU.add, [r_tmp, j["r_bc"]], [r_dst])
                tok0 = j["tok0"]
                if j["final"]:
                    P.dma(out_d[tok0:tok0 + 128, :], dst, reads=[r_dst], writes=[r_out])
                    return
                P.dma(hres_d[tok0:tok0 + 128, :], dst, reads=[r_dst], writes=[r_hres])
                xbf, r_xbf = j["xbf"], j["r_xbf"]
                actf(xbf, dst, AF.Copy, [r_dst], [r_xbf])

            def phD(self, j):
                if j["final"]:
                    return
                xbf, r_xbf = j["xbf"], j["r_xbf"]
                for i2 in range(2):
                    bank = j["pbank"][i2]
                    pb = psb(bank).rearrange("p (a b) -> p a b", b=128)
                    for k4 in range(4):
                        kc = i2 * 4 + k4
                        mm(pb[:, k4, :], xbf[:, kc * 128:(kc + 1) * 128], ident[:], True, True, [r_xbf, r_ident], [r_ps[bank]])

            def phE(self, j):
                if j["final"]:
                    return
                hTs, r_hTs, jj = j["hTs"], j["r_hTs"], j["j"]
                for i2 in range(2):
                    bank = j["pbank"][i2]
                    pb = psb(bank).rearrange("p (a b) -> p a b", b=128)
                    P.op("act", lambda e, pb=pb, i2=i2: e.activation(out=hTs[:, i2 * 4:(i2 + 1) * 4, jj * 128:(jj + 1) * 128], in_=pb,
                                                                  func=AF.Copy), [r_ps[bank]], [r_hTs])
                if jj == 3:
                    t0 = j["tok0"] - jj * 128
                    P.dma(hT_d[:, t0:t0 + 512].rearrange("(kc p) n -> p kc n", p=128), hTs, reads=[r_hTs], writes=[r_hT])

        def stage0():
            m0 = A.mark()
            bc0 = A.f32(2 * D)
            r_bc0 = P.res("bc0")
            P.dma(bc0, bc0_d, writes=[r_bc0])
            xt = [A.f32(D) for _ in range(4)]
            r_xt = [P.res("xt%d" % i) for i in range(4)]
            tmp = [A.f32(D) for _ in range(2)]
            r_tmp = [P.res("tmp%d" % i) for i in range(2)]
            ht = [A.f32(D) for _ in range(2)]
            r_ht = [P.res("ht%d" % i) for i in range(2)]
            xbf = [A.bf16(D) for _ in range(2)]
            r_xbf = [P.res("xbf%d" % i) for i in range(2)]
            hTs = [A.bf16(8 * 512).rearrange("p (a b) -> p a b", b=512) for _ in range(2)]
            r_hTs = [P.res("hTs%d" % i, multi=True) for i in range(2)]
            pipe = LNPipe()
            for pb_ in range(2):
                P.dma(xt[pb_], x_d[pb_ * 128:(pb_ + 1) * 128, :], writes=[r_xt[pb_]])
            for blk in range(T // 128):
                k = blk % 2
                x4 = blk % 4
                nb_ = blk + 2
                if nb_ < T // 128:
                    P.dma(xt[nb_ % 4], x_d[nb_ * 128:(nb_ + 1) * 128, :], writes=[r_xt[nb_ % 4]])
                sg_ = (blk // 4) % 2
                pipe.push(dict(src=xt[x4], r_src=r_xt[x4], dst=ht[k], r_dst=r_ht[k], g=bc0[:, 0:D], b=bc0[:, D:2 * D], r_bc=r_bc0,
                               tmp=tmp[k], r_tmp=r_tmp[k], tok0=blk * 128, final=False, xbf=xbf[k], r_xbf=r_xbf[k],
                               hTs=hTs[sg_], r_hTs=r_hTs[sg_], j=blk % 4, pbank=(2 * k, 2 * k + 1)))
            pipe.flush()
            P.barrier()
            A.release(m0)

        def load_bcl(l):
            bcl = A.f32(BCL)
            r = P.res("bcl")
            P.dma(bcl, bcl_d[l], writes=[r])
            return bcl, r

        def stage1(l, qT, kT, vsb, r_qT, r_kT, r_v):
            m0 = A.mark()
            bcl, r_bcl = load_bcl(l)
            hTh = A.bf16(8 * TH).rearrange("p (a b) -> p a b", b=TH)
            W = [A.bf16(8 * 512).rearrange("p (a b) -> p a b", b=512) for _ in range(2)]
            r_W = [P.res("W%d" % i) for i in range(2)]
            cst = [A.f32(2 * 512).rearrange("p (a b) -> p a b", b=512) for _ in range(2)]
            r_cst = [P.res("cst%d" % i) for i in range(2)]
            t1 = [A.f32(512) for _ in range(2)]
            t2 = [A.f32(512) for _ in range(2)]
            r_t1 = [P.res("t1_%d" % i) for i in range(2)]
            r_t2 = [P.res("t2_%d" % i) for i in range(2)]
            xs = [A.f32(512) for _ in range(2)]
            r_xs = [P.res("xs%d" % i) for i in range(2)]
            gs = [A.bf16(4 * 512).rearrange("p (a b) -> p a b", b=512) for _ in range(2)]
            r_gs = [P.res("gs%d" % i, multi=True) for i in range(2)]
            memset("pool", vsb[:, :, :, 128:130], 1.0, [r_v])
            bv_bc = bcl[:, 5 * 1024:5 * 1024 + 512].rearrange("p (h e) -> p h e", e=128)
            wi = [0]
            ci = [0]
            xi = [0]
            gi = [0]
            r_hThc = [P.res("hTh_c%d" % i) for i in range(4)]

            def load_hTh(half_, c_):
                t0_ = half_ * TH + c_ * 512
                P.dma(hTh[:, :, c_ * 512:(c_ + 1) * 512], hT_d[:, t0_:t0_ + 512].rearrange("(kc p) n -> p kc n", p=128),
                      reads=[r_hT], writes=[r_hThc[c_]])

            sched = []
            for half in range(2):
                groups = list(range(11)) if (l == 0 or half == 0) else list(range(6))
                sched += [(half, g) for g in groups]

            def load_W(idx_):
                g_ = sched[idx_][1]
                P.dma(W[idx_ % 2], win_d[l][:, g_ * 512:(g_ + 1) * 512].rearrange("(kc p) n -> p kc n", p=128),
                      writes=[r_W[idx_ % 2]], eng="pool")

            load_W(0)
            for c_ in range(4):
                load_hTh(0, c_)
            for idx, (half, g) in enumerate(sched):
                if True:
                    if idx + 1 < len(sched):
                        load_W(idx + 1)
                    wk = idx % 2
                    last_of_half = idx + 1 < len(sched) and sched[idx + 1][0] != half
                    Wg = W[wk]
                    rW = r_W[wk]
                    for tt_ in range(4):
                        r_hTh = r_hThc[tt_]
                        tok0 = half * TH + tt_ * 512
                        if g < 4:
                            if l == 1 and half == 1:
                                chunks = [2, 3]
                            else:
                                chunks = [0, 1, 2, 3]
                            ck = ci[0] % 2
                            ci[0] += 1
                            P.dma(cst[ck], cs_d[:, :, tok0:tok0 + 512], writes=[r_cst[ck]])
                            for c in chunks:
                                for kc in range(8):
                                    mm(psb(c), Wg[:, kc, c * 128:(c + 1) * 128], hTh[:, kc, tt_ * 512:(tt_ + 1) * 512],
                                       kc == 0, kc == 7, [rW, r_hTh], [r_ps[c]])
                            for (c0, dst, r_dst) in ((0, qT, r_qT), (2, kT, r_kT)):
                                if c0 not in chunks:
                                    continue
                                k2 = (c0 // 2)
                                stt("dve", t1[k2], psb(c0), cp(l, g * 4 + c0), cst[ck][:, 0, :], ALU.add, ALU.mult,
                                    [r_ps[c0], r_colp, r_cst[ck]], [r_t1[k2]])
                                stt("dve", t2[k2], psb(c0 + 1), cp(l, g * 4 + c0 + 1), cst[ck][:, 1, :], ALU.add, ALU.mult,
                                    [r_ps[c0 + 1], r_colp, r_cst[ck]], [r_t2[k2]])
                                tt("pool", dst[:, g, tok0:tok0 + 512], t1[k2], t2[k2], ALU.add,
                                   [r_t1[k2], r_t2[k2]], [r_dst])
                        elif g == 4:
                            for j in range(4):
                                blk = tok0 // 128 + j
                                bank = 4 + (j % 2)
                                for kc in range(8):
                                    mm(psb(bank), hTh[:, kc, tt_ * 512 + j * 128:tt_ * 512 + (j + 1) * 128], Wg[:, kc, :],
                                       kc == 0, kc == 7, [rW, r_hTh], [r_ps[bank]])
                                tt("dve", vsb[:, blk, :, 0:128], psb(bank).rearrange("p (h e) -> p h e", e=128), bv_bc, ALU.add,
                                   [r_ps[bank], r_bcl], [r_v])
                        elif g == 5:
                            for c in range(4):
                                bank = 4 + (c % 2)
                                for kc in range(8):
                                    mm(psb(bank), Wg[:, kc, c * 128:(c + 1) * 128], hTh[:, kc, tt_ * 512:(tt_ + 1) * 512],
                                       kc == 0, kc == 7, [rW, r_hTh], [r_ps[bank]])
                                xk = xi[0] % 2
                                xi[0] += 1
                                actf(xs[xk], psb(bank), AF.Identity, [r_ps[bank], r_colp], [r_xs[xk]], bias=cp(l, 20 + c))
                                P.dma(xr_d[c * 128:(c + 1) * 128, tok0:tok0 + 512], xs[xk], reads=[r_xs[xk]], writes=[r_xr])
                        else:
                            func = AF.Gelu if g == 6 else AF.Sigmoid
                            gk = gi[0] % 2
                            gi[0] += 1
                            for c in range(4):
                                bank = 4 + (c % 2)
                                for kc in range(8):
                                    mm(psb(bank), Wg[:, kc, c * 128:(c + 1) * 128], hTh[:, kc, tt_ * 512:(tt_ + 1) * 512],
                                       kc == 0, kc == 7, [rW, r_hTh], [r_ps[bank]])
                                actf(gs[gk][:, c, :], psb(bank), func, [r_ps[bank], r_colp], [r_gs[gk]], bias=cp(l, g * 4 + c))
                            row0 = (g - 6) * 512
                            P.dma(gates_d[row0:row0 + 512, tok0:tok0 + 512].rearrange("(c p) n -> p c n", p=128), gs[gk],
                                  reads=[r_gs[gk]], writes=[r_gates])
                        if last_of_half:
                            load_hTh(half + 1, tt_)
            P.barrier()
            A.release(m0)

        def stage3(l):
            m0 = A.mark()
            wbd = A.bf16(16 * 128).rearrange("p (a b) -> p a b", b=128)
            r_wbd = P.res("wbd")
            P.dma(wbd, rgbd_d[l].rearrange("m p n -> p m n"), writes=[r_wbd], eng="pool")
            cneg = A.f32(8)
            r_cneg = P.res("cneg")
            lamc = colp[:, l * NCOLP + 84:l * NCOLP + 92]
            actf(cneg, lamc, AF.Exp, [r_colp], [r_cneg], scale=-1.0)
            actf(cneg, cneg, AF.Ln, [r_cneg], [r_cneg], bias=1.0)
            ts("dve", cneg, cneg, -8.0, None, ALU.mult, None, [r_cneg], [r_cneg])
            chalf = A.f32(8)
            ts("dve", chalf, cneg, 0.5, None, ALU.mult, None, [r_cneg], [r_cneg])
            hbias = A.f32(16)
            ts("dve", hbias, colp[:, l * NCOLP + 68:l * NCOLP + 84], 0.5, None, ALU.mult, None, [r_colp], [r_cneg])
            xrt = A.f32(T + 4)
            r_xrt = [P.res("xrtA"), P.res("xrtB")]
            xc = A.f32(T)
            r_xc = [P.res("xcA"), P.res("xcB")]
            xcb = A.bf16(T)
            r_xcb = P.res("xcb")
            hf = A.f32(T)
            r_hf = P.res("hf")
            TW = 1024
            hbt = [A.f32(TW) for _ in range(2)]
            r_hbt = [P.res("hbt%d" % i) for i in range(2)]
            NB = 2
            rr = [A.f32(TW) for _ in range(NB)]
            aa = [A.f32(TW) for _ in range(NB)]
            ii = rr
            gg = [A.f32(TW) for _ in range(NB)]
            sq = [A.f32(TW) for _ in range(NB)]
            r_rr = [P.res("rr%d" % i) for i in range(NB)]
            r_aa = [P.res("aa%d" % i) for i in range(NB)]
            r_ii = r_rr
            r_gg = [P.res("gg%d" % i) for i in range(NB)]
            r_sq = [P.res("sq%d" % i) for i in range(NB)]
            ybs = [A.bf16(TW) for _ in range(2)]
            r_ybs = [P.res("ybs%d" % i) for i in range(2)]
            NT = T // TW
            cnt = 0
            memset("pool", xrt[:, 0:2], 0.0, [r_xrt[0]])
            memset("pool", xrt[:, T + 2:T + 4], 0.0, [r_xrt[1]])
            P.dma(xrt[:, 2:T + 2], xr_d[0:128, :], reads=[r_xr], writes=r_xrt)
            for c in range(4):
                for hh, eng in ((0, "dve"), (1, "dve")):
                    o0 = hh * TH
                    dst = xc[:, o0:o0 + TH]
                    ts(eng, dst, xrt[:, o0:o0 + TH], cp(l, 44 + c), cp(l, 64 + c), ALU.mult, ALU.add,
                       r_xrt + [r_colp], [r_xc[hh]])
                    for j in range(1, 5):
                        stt(eng, dst, xrt[:, o0 + j:o0 + j + TH], cp(l, 44 + j * 4 + c), dst, ALU.mult, ALU.add,
                            r_xrt + [r_colp, r_xc[hh]], [r_xc[hh]])
                actf(xcb, xc, AF.Copy, r_xc, [r_xcb])
                if c + 1 < 4:
                    P.dma(xrt[:, 2:T + 2], xr_d[(c + 1) * 128:(c + 2) * 128, :], reads=[r_xr], writes=r_xrt)
                its = [(0, ti, tl) for ti, tl in enumerate(range(NT))] + [(1, ti, tl) for ti, tl in enumerate(range(NT - 1, -1, -1))]

                def ph1(i):
                    d, ti, tl = its[i]
                    k = i % 2
                    sl = slice(tl * TW, (tl + 1) * TW)
                    b0 = 4 * (i % 2)
                    for h2 in range(2):
                        s2 = slice(tl * TW + h2 * 512, tl * TW + (h2 + 1) * 512)
                        mm(psb(b0 + h2), wbd[:, d * 8 + 0 * 4 + c, :], xcb[:, s2], True, True, [r_wbd, r_xcb], [r_ps[b0 + h2]])
                        mm(psb(b0 + 2 + h2), wbd[:, d * 8 + 1 * 4 + c, :], xcb[:, s2], True, True, [r_wbd, r_xcb], [r_ps[b0 + 2 + h2]])
                    ch_ = chalf[:, d * 4 + c:d * 4 + c + 1]
                    rr3 = rr[k].rearrange("p (a b) -> p a b", b=512)
                    actf(rr3, ps[:, b0:b0 + 2, :], AF.Tanh, [r_ps[b0], r_ps[b0 + 1], r_cneg], [r_rr[k]],
                         bias=hbias[:, d * 4 + c:d * 4 + c + 1], scale=0.5)
                    actf(aa[k], rr[k], AF.Exp, [r_rr[k], r_cneg], [r_aa[k]], scale=ch_, bias=ch_)
                    tt("pool", sq[k], aa[k], aa[k], ALU.mult, [r_aa[k]], [r_sq[k]])
                    actf(rr3, ps[:, b0 + 2:b0 + 4, :], AF.Tanh, [r_ps[b0 + 2], r_ps[b0 + 3], r_cneg], [r_ii[k]],
                         bias=hbias[:, 8 + d * 4 + c:9 + d * 4 + c], scale=0.5)
                    ts("dve", sq[k], sq[k], 1.0, -1.0, ALU.min, ALU.mult, [r_sq[k]], [r_sq[k]])
                    stt("dve", gg[k], ii[k], 1.0, xc[:, sl], ALU.add, ALU.mult, [r_ii[k]] + r_xc, [r_gg[k]])

                def ph2(i):
                    d, ti, tl = its[i]
                    k = i % 2
                    sl = slice(tl * TW, (tl + 1) * TW)
                    actf(sq[k], sq[k], AF.Sqrt, [r_sq[k]], [r_sq[k]], bias=1.0)
                    stt("dve", gg[k], sq[k], 0.5, gg[k], ALU.mult, ALU.mult, [r_sq[k], r_gg[k]], [r_gg[k]])
                    if d == 0:
                        init = 0.0 if ti == 0 else hf[:, tl * TW - 1:tl * TW]
                        o_ap, a_ap, b_ap = hf[:, sl], aa[k], gg[k]
                        rdx, wrx = [r_aa[k], r_gg[k], r_hf], [r_hf]
                    else:
                        hk = ti % 2
                        init = 0.0 if ti == 0 else hbt[1 - hk][:, 0:1]
                        o_ap, a_ap, b_ap = hbt[hk][:, ::-1], aa[k][:, ::-1], gg[k][:, ::-1]
                        rdx, wrx = [r_aa[k], r_gg[k], r_hbt[1 - hk]], [r_hbt[hk]]
                    P.op("dve", lambda e, o_ap=o_ap, a_ap=a_ap, b_ap=b_ap, init=init:
                         e.tensor_tensor_scan(o_ap, a_ap, b_ap, init, ALU.mult, ALU.add), rdx, wrx)
                    if d == 1:
                        yk = ti % 2
                        tt("pool", ybs[yk], hf[:, sl], hbt[hk], ALU.add, [r_hf, r_hbt[hk]], [r_ybs[yk]])
                        P.dma(yb_d[c * 128:(c + 1) * 128, sl], ybs[yk], reads=[r_ybs[yk]], writes=[r_yb])

                for i in range(len(its)):
                    ph1(i)
                    if i >= 1:
                        ph2(i - 1)
                ph2(len(its) - 1)
            P.barrier()
            A.release(m0)

        def stage2(l, qT, kT, vsb, r_qT, r_kT, r_v, nqt):
            m0 = A.mark()
            bcl, r_bcl = load_bcl(l)
            lambda_init = 0.8 - 0.6 * math.exp(-0.3 * l)
            lamt = A.f32(256)
            r_lam = P.res("lam")
            lams = A.f32(8)
            lq = bcl[:, 5 * 1024 + 512 + 128:5 * 1024 + 512 + 128 + 256]
            tt("dve", lamt[:, 0:64], lq[:, 0:64], lq[:, 64:128], ALU.mult, [r_bcl], [r_lam])
            tt("dve", lamt[:, 64:128], lq[:, 128:192], lq[:, 192:256], ALU.mult, [r_bcl, r_lam], [r_lam])
            P.op("dve", lambda e: e.reduce_sum(lams[:, 0:2], lamt[:, 0:128].rearrange("p (a b) -> p a b", b=64), axis=AX.X),
                 [r_lam], [r_lam])
            actf(lams[:, 2:4], lams[:, 0:2], AF.Exp, [r_lam], [r_lam])
            tt("dve", lams[:, 4:5], lams[:, 3:4], lams[:, 2:3], ALU.subtract, [r_lam], [r_lam])
            ts("dve", lams[:, 5:6], lams[:, 4:5], -lambda_init, None, ALU.add, None, [r_lam], [r_lam])
            neg_lam = lams[:, 5:6]
            g_bc = bcl[:, 5 * 1024 + 512:5 * 1024 + 512 + 128]
            PT = [A.bf16(2 * 512).rearrange("p (a b) -> p a b", b=512) for _ in range(3)]
            r_PT = [P.res("PT%d" % i) for i in range(3)]
            otok = A.bf16(4 * 512).rearrange("p (a b) -> p a b", b=512)
            r_otok = P.res("otok", multi=True)
            ats = [A.bf16(4 * 512).rearrange("p (a b) -> p a b", b=512) for _ in range(2)]
            r_ats = [P.res("ats%d" % i, multi=True) for i in range(2)]
            o1 = A.f32(128)
            o2 = A.f32(128)
            osq = A.f32(128)
            fin = A.f32(16)
            r_fin = P.res("fin")
            accs = [A.f32(4 * 264).rearrange("p (a b) -> p a b", b=264) for _ in range(2)]
            r_accs = [P.res("accs%d" % i, multi=True) for i in range(2)]

            def acc(m, qb):
                bank = 4 + 2 * m + qb // 2
                off = (qb % 2) * 132
                return ps[:, bank, off:off + 129], bank

            iters = [(qt, h, kc) for qt in range(nqt) for h in range(4) for kc in range(32)]
            NI = len(iters)

            def emit_S(i):
                qt, h, kc = iters[i]
                b0 = 2 * (i % 2)
                for m in range(2):
                    mm(psb(b0 + m), kT[m * 64:(m + 1) * 64, h, kc * 128:(kc + 1) * 128],
                       qT[m * 64:(m + 1) * 64, h, qt * 512:(qt + 1) * 512], True, True,
                       [r_kT, r_qT], [r_ps[b0 + m]])

            def emit_exp(i):
                b0 = 2 * (i % 2)
                pk = i % 3
                actf(PT[pk], ps[:, b0:b0 + 2, :], AF.Exp, [r_ps[b0], r_ps[b0 + 1]], [r_PT[pk]], scale=0.125)

            def emit_PV(i):
                qt, h, kc = iters[i]
                pk = i % 3
                for m in range(2):
                    for qb in range(4):
                        a_ap, bank = acc(m, qb)
                        mm(a_ap, PT[pk][:, m, qb * 128:(qb + 1) * 128], vsb[:, kc, h, 0:129],
                           kc == 0 and qb % 2 == 0, kc == 31, [r_PT[pk], r_v], [r_ps[bank]], sgc=True)

            fin2 = [A.f32(32) for _ in range(2)]
            r_fin2 = [P.res("fin2_%d" % i) for i in range(2)]
            o2b = [A.f32(4 * 128).rearrange("p (a b) -> p a b", b=128) for _ in range(2)]

            def emit_finA(fk):
                ac = accs[fk]
                r_ac = r_accs[fk]
                f = fin2[fk]
                rf = r_fin2[fk]
                for bb in range(4):
                    P.op("dve", lambda e, bb=bb: e.tensor_copy(ac[:, bb, :], ps[:, 4 + bb, 0:264]), [r_ps[4 + bb]], [r_ac])
                P.op("dve", lambda e: e.reciprocal(f[:, 0:4].rearrange("p (a b) -> p a b", b=2), ac[:, 0:2, 128:261:132]), [r_ac], [rf])
                P.op("dve", lambda e: e.reciprocal(f[:, 4:8].rearrange("p (a b) -> p a b", b=2), ac[:, 2:4, 128:261:132]), [r_ac], [rf])
                ts("dve", f[:, 4:8], f[:, 4:8], neg_lam, None, ALU.mult, None, [rf, r_lam], [rf])
                for qb in range(4):
                    off = (qb % 2) * 132
                    a0 = ac[:, qb // 2, off:off + 128]
                    a1 = ac[:, 2 + qb // 2, off:off + 128]
                    ts("dve", o1, a0, f[:, qb:qb + 1], None, ALU.mult, None, [r_ac, rf], [r_fin])
                    stt("dve", o2b[fk][:, qb, :], a1, f[:, 4 + qb:5 + qb], o1, ALU.mult, ALU.add, [r_ac, rf, r_fin], [rf])
                    tt("dve", osq, o2b[fk][:, qb, :], o2b[fk][:, qb, :], ALU.mult, [rf], [r_fin])
                    P.op("dve", lambda e, qb=qb: e.reduce_sum(f[:, 8 + qb:9 + qb], osq, axis=AX.X), [r_fin], [rf])

            def emit_finBC(h, fk):
                f = fin2[fk]
                rf = r_fin2[fk]
                actf(f[:, 12:16], f[:, 8:12], AF.Ln, [rf], [rf], bias=EPS, scale=1.0 / 128.0)
                actf(f[:, 16:20], f[:, 12:16], AF.Exp, [rf], [rf], scale=-0.5)
                ts("dve", f[:, 16:20], f[:, 16:20], 1.0 - lambda_init, None, ALU.mult, None, [rf], [rf])
                for qb in range(4):
                    stt("dve", otok[:, qb, h * 128:(h + 1) * 128], o2b[fk][:, qb, :], f[:, 16 + qb:17 + qb], g_bc, ALU.mult, ALU.mult,
                        [rf, r_bcl], [r_otok])

            def emit_T(qt, half, bank0):
                ak = qt % 2
                for q2 in range(2):
                    qb = 2 * half + q2
                    bank = bank0 + q2
                    pb = psb(bank).rearrange("p (a b) -> p a b", b=128)
                    for c in range(4):
                        mm(pb[:, c, :], otok[:, qb, c * 128:(c + 1) * 128], ident[:], True, True, [r_otok, r_ident], [r_ps[bank]])
                    P.op("dve", lambda e, pb=pb, qb=qb, ak=ak: e.tensor_copy(ats[ak][:, :, qb * 128:(qb + 1) * 128], pb),
                         [r_ps[bank]], [r_ats[ak]])
                if half == 1:
                    P.dma(att_d[:, qt * 512:(qt + 1) * 512].rearrange("(c p) n -> p c n", p=128), ats[ak],
                          reads=[r_ats[ak]], writes=[r_att])

            pending_T = None
            pending_F = None
            fcnt = 0
            emit_S(0)
            emit_S(1)
            for i in range(NI):
                qt, h, kc = iters[i]
                emit_exp(i)
                if pending_T is not None and kc in (12, 13):
                    emit_T(pending_T, kc - 12, 2 * (i % 2))
                    if kc == 13:
                        pending_T = None
                if i + 2 < NI:
                    emit_S(i + 2)
                emit_PV(i)
                if kc == 31:
                    emit_finA(fcnt % 2)
                    pending_F = (qt, h, fcnt % 2)
                    fcnt += 1
                if kc == 6 and pending_F is not None:
                    emit_finBC(pending_F[1], pending_F[2])
                    if pending_F[1] == 3:
                        pending_T = pending_F[0]
                    pending_F = None
            if pending_F is not None:
                emit_finBC(pending_F[1], pending_F[2])
                if pending_F[1] == 3:
                    pending_T = pending_F[0]
            if pending_T is not None:
                emit_T(pending_T, 0, 0)
                emit_T(pending_T, 1, 2)
            P.barrier()
            A.release(m0)

        def stage4(l, bcl, r_bcl, ntiles):
            m0 = A.mark()
            wpa = A.bf16(4 * D).rearrange("p (a b) -> p a b", b=D)
            wpb = A.bf16(4 * D).rearrange("p (a b) -> p a b", b=D)
            wo = A.bf16(8 * D).rearrange("p (a b) -> p a b", b=D)
            r_wpa, r_wpb, r_wo = P.res("wpa"), P.res("wpb"), P.res("wo")
            P.dma(wpa, wpa_d[l].rearrange("(kc p) n -> p kc n", p=128), writes=[r_wpa], eng="pool")
            P.dma(wpb, wpb_d[l].rearrange("(kc p) n -> p kc n", p=128), writes=[r_wpb], eng="pool")
            P.dma(wo, wo_d[l].rearrange("(kc p) n -> p kc n", p=128), writes=[r_wo], eng="pool")
            att_t = [A.bf16(4 * 512).rearrange("p (a b) -> p a b", b=512) for _ in range(2)]
            yb_t = [A.bf16(4 * 512).rearrange("p (a b) -> p a b", b=512) for _ in range(2)]
            g_t = [A.bf16(20 * 512).rearrange("p (a b) -> p a b", b=512) for _ in range(2)]
            h_t1 = A.f32(4 * D).rearrange("p (a b) -> p a b", b=D)
            h_t = [h_t1, h_t1]
            r_htb = [P.res("s4h%d" % i) for i in range(4)]
            r_in = [P.res("s4in%d" % i, multi=True) for i in range(2)]
            ybg = A.bf16(4 * 512).rearrange("p (a b) -> p a b", b=512)
            r_ybg = P.res("ybg")
            merged = A.bf16(8 * 512).rearrange("p (a b) -> p a b", b=512)
            r_merged = P.res("merged", multi=True)
            tA = [A.f32(512) for _ in range(2)]
            tB = [A.f32(512) for _ in range(2)]
            r_tA = [P.res("tA%d" % i) for i in range(2)]
            r_tB = [P.res("tB%d" % i) for i in range(2)]
            x1 = [A.f32(D) for _ in range(2)]
            r_x1 = [P.res("x1_%d" % i) for i in range(2)]
            tmp = [A.f32(D) for _ in range(2)]
            r_tmp = [P.res("tmp%d" % i) for i in range(2)]
            h1 = [A.f32(D) for _ in range(2)]
            r_h1 = [P.res("h1_%d" % i) for i in range(2)]
            xbf = [A.bf16(D) for _ in range(2)]
            r_xbf = [P.res("xbf%d" % i) for i in range(2)]
            hTs = [A.bf16(8 * 512).rearrange("p (a b) -> p a b", b=512) for _ in range(2)]
            r_hTs = [P.res("hTs%d" % i, multi=True) for i in range(2)]
            g1 = bcl[:, 0:D]
            b1 = bcl[:, D:2 * D]
            bo = bcl[:, 4 * D:5 * D]
            EPS2 = EPS / (ALPHA * ALPHA)
            bos = A.f32(D)
            r_bos = P.res("bos")
            ts("dve", bos, bo, 1.0 / ALPHA, None, ALU.mult, None, [r_bcl], [r_bos])
            bi = 0
            pipe = LNPipe()

            def finish(pd):
                (tok0_, blk_, b2_, k_) = pd
                ln_back(h1[b2_], r_h1[b2_], b1, r_bcl, tmp[b2_], r_tmp[b2_])
                emit_h(tok0_ + blk_ * 128, h1[b2_], r_h1[b2_], xbf[b2_], r_xbf[b2_], hTs[k_], r_hTs[k_], blk_, 4,
                       (2 * b2_, 2 * b2_ + 1))

            def load_in(tl_):
                k_ = tl_ % 2
                t0_ = tl_ * 512
                P.dma(att_t[k_], att_d[:, t0_:t0_ + 512].rearrange("(c p) n -> p c n", p=128), reads=[r_att], writes=[r_in[k_]])
                P.dma(yb_t[k_], yb_d[:, t0_:t0_ + 512].rearrange("(c p) n -> p c n", p=128), reads=[r_yb], writes=[r_in[k_]])
                P.dma(g_t[k_], gates_d[:, t0_:t0_ + 512].rearrange("(c p) n -> p c n", p=128), reads=[r_gates], writes=[r_in[k_]])

            load_in(0)
            for tl in range(ntiles):
                k = tl % 2
                tok0 = tl * 512
                if tl + 1 < ntiles:
                    load_in(tl + 1)
                for blk in range(4):
                    P.dma(h_t[k][:, blk, :], hres_d[tok0 + blk * 128:tok0 + (blk + 1) * 128, :], reads=[r_hres], writes=[r_htb[blk]])
                tt("pool", ybg, yb_t[k], g_t[k][:, 0:4, :], ALU.mult, [r_in[k]], [r_ybg])
                for blk in range(4):
                    tt("pool", h_t[k][:, blk, :], h_t[k][:, blk, :], bos, ALU.add, [r_htb[blk], r_bos], [r_htb[blk]])
                for fc in range(8):
                    f2 = fc % 2
                    bA, bB = 2 * f2, 2 * f2 + 1
                    for kc in range(4):
                        mm(psb(bA), wpa[:, kc, fc * 128:(fc + 1) * 128], att_t[k][:, kc, :], kc == 0, kc == 3,
                           [r_wpa, r_in[k]], [r_ps[bA]])
                    for kc in range(4):
                        mm(psb(bB), wpb[:, kc, fc * 128:(fc + 1) * 128], ybg[:, kc, :], kc == 0, kc == 3,
                           [r_wpb, r_ybg], [r_ps[bB]])
                    tt("dve", tA[f2], psb(bA), g_t[k][:, 4 + fc, :], ALU.mult, [r_ps[bA], r_in[k]], [r_tA[f2]])
                    tt("dve", tB[f2], psb(bB), g_t[k][:, 12 + fc, :], ALU.mult, [r_ps[bB], r_in[k]], [r_tB[f2]])
                    tt("dve", merged[:, fc, :], tA[f2], tB[f2], ALU.add, [r_tA[f2], r_tB[f2]], [r_merged])
                for blk in range(4):
                    b2 = bi % 2
                    bi += 1
                    for hh in range(2):
                        bank = 2 * b2 + hh
                        for kc in range(8):
                            mm(psb(bank), merged[:, kc, blk * 128:(blk + 1) * 128], wo[:, kc, hh * 512:(hh + 1) * 512],
                               kc == 0, kc == 7, [r_merged, r_wo], [r_ps[bank]])
                        stt("dve", x1[b2][:, hh * 512:(hh + 1) * 512], psb(bank), 1.0 / ALPHA, h_t[k][:, blk, hh * 512:(hh + 1) * 512],
                            ALU.mult, ALU.add, [r_htb[blk], r_ps[bank]], [r_x1[b2]])
                    pipe.push(dict(src=x1[b2], r_src=r_x1[b2], dst=h1[b2], r_dst=r_h1[b2], g=g1, b=b1, r_bc=r_bcl,
                                   tmp=tmp[b2], r_tmp=r_tmp[b2], tok0=tok0 + blk * 128, final=False, xbf=xbf[b2], r_xbf=r_xbf[b2],
                                   hTs=hTs[k], r_hTs=r_hTs[k], j=blk, pbank=(4 + 2 * b2, 5 + 2 * b2), eps=EPS2))
            pipe.flush()
            P.barrier()
            A.release(m0)

        def stage5(l, bcl, r_bcl, tok_base, final):
            m0 = A.mark()
            moe = (l % 2 == 1)
            hTh = A.bf16(8 * TH).rearrange("p (a b) -> p a b", b=TH)
            r_hThc = [P.res("hTh5_%d" % i) for i in range(4)]
            for c_ in range(4):
                t0_ = tok_base + c_ * 512
                P.dma(hTh[:, :, c_ * 512:(c_ + 1) * 512], hT_d[:, t0_:t0_ + 512].rearrange("(kc p) n -> p kc n", p=128),
                      reads=[r_hT], writes=[r_hThc[c_]])
            yacc = A.f32(16 * D).rearrange("p (a b) -> p a b", b=D)
            r_y = [P.res("yacc%d" % i) for i in range(16)]
            for q4 in range(4):
                P.dma(yacc[:, q4 * 4:(q4 + 1) * 4, :],
                      hres_d[tok_base + q4 * 512:tok_base + (q4 + 1) * 512, :].rearrange("(b p) d -> p b d", p=128),
                      reads=[r_hres], writes=r_y[q4 * 4:(q4 + 1) * 4])
            comb = None
            r_comb = P.res("comb", multi=True)
            if moe:
                j = l // 2
                wr = A.bf16(8 * NE).rearrange("p (a b) -> p a b", b=NE)
                r_wr = P.res("wr")
                P.dma(wr, mwr_d[j].rearrange("(kc p) n -> p kc n", p=128), writes=[r_wr], eng="pool")
                comb = A.f32(16 * NE).rearrange("p (a b) -> p a b", b=NE)
                lg = A.f32(NE)
                m8 = A.f32(8)
                ex = A.f32(NE)
                msk = A.f32(NE)
                sc = A.f32(8)
                r_rt = P.res("router")
                br_bc = bcl[:, 5 * 1024 + 512 + 128 + 256:5 * 1024 + 512 + 128 + 256 + NE]
                for b in range(16):
                    bank = b % 2
                    for kc in range(8):
                        mm(ps[:, bank, 0:NE], hTh[:, kc, b * 128:(b + 1) * 128], wr[:, kc, :], kc == 0, kc == 7,
                           [r_hThc[b // 4], r_wr], [r_ps[bank]])
                    tt("dve", lg, ps[:, bank, 0:NE], br_bc, ALU.add, [r_ps[bank], r_bcl], [r_rt])
                    P.op("dve", lambda e: e.max(out=m8, in_=lg), [r_rt], [r_rt])
                    ts("dve", sc[:, 0:1], m8[:, 0:1], -1.0, None, ALU.mult, None, [r_rt], [r_rt])
                    ts("dve", msk, lg, m8[:, 1:2], None, ALU.is_ge, None, [r_rt], [r_rt])
                    actf(ex, lg, AF.Exp, [r_rt], [r_rt], bias=sc[:, 0:1])
                    tt("dve", ex, ex, msk, ALU.mult, [r_rt], [r_rt])
                    P.op("dve", lambda e: e.reduce_sum(sc[:, 1:2], ex, axis=AX.X), [r_rt], [r_rt])
                    P.op("dve", lambda e: e.reciprocal(sc[:, 2:3], sc[:, 1:2]), [r_rt], [r_rt])
                    ts("dve", comb[:, b, :], ex, sc[:, 2:3], 1.0 / ALPHA, ALU.mult, ALU.mult, [r_rt], [r_comb])
                experts = [(mwg_d[j][e], mwu_d[j][e], mwd_d[j][e], e, DFE) for e in range(NE)]
            else:
                j = l // 2
                experts = [(fwg_d[j], fwu_d[j], fwd_d[j], None, DFF)]
            wg = [A.bf16(8 * 512).rearrange("p (a b) -> p a b", b=512) for _ in range(2)]
            wu = [A.bf16(8 * 512).rearrange("p (a b) -> p a b", b=512) for _ in range(2)]
            wd = [A.bf16(4 * D).rearrange("p (a b) -> p a b", b=D) for _ in range(2)]
            r_wk = [P.res("w5_%d" % i, multi=True) for i in range(2)]
            aT = [A.bf16(4 * 512).rearrange("p (a b) -> p a b", b=512) for _ in range(2)]
            r_aT = [P.res("aT%d" % i, multi=True) for i in range(2)]
            sg = [A.f32(512) for _ in range(2)]
            r_sg = [P.res("sg%d" % i) for i in range(2)]
            g2 = bcl[:, 2 * D:3 * D]
            b2_ = bcl[:, 3 * D:4 * D]
            tmp = [A.f32(D) for _ in range(2)]
            r_tmp = [P.res("tmp5_%d" % i) for i in range(2)]
            h2 = [A.f32(D) for _ in range(2)]
            r_h2 = [P.res("h2_%d" % i) for i in range(2)]
            if not final:
                xbf1 = A.bf16(D)
                xbf = [xbf1, xbf1]
                r_xbf1 = P.res("xbf5")
                r_xbf = [r_xbf1, r_xbf1]
                hTs1 = A.bf16(8 * 512).rearrange("p (a b) -> p a b", b=512)
                hTs = [hTs1, hTs1]
                r_hTs1 = P.res("hTs5", multi=True)
                r_hTs = [r_hTs1, r_hTs1]
            pipe = LNPipe()

            def push_block(b):
                k = b % 2
                job = dict(src=yacc[:, b, :], r_src=r_y[b], dst=h2[k], r_dst=r_h2[k], g=g2, b=b2_, r_bc=r_bcl,
                           tmp=tmp[k], r_tmp=r_tmp[k], tok0=tok_base + b * 128, final=final, eps=EPS / (ALPHA * ALPHA))
                if not final:
                    sg_ = (b // 4) % 2
                    job.update(xbf=xbf[k], r_xbf=r_xbf[k], hTs=hTs[sg_], r_hTs=r_hTs[sg_], j=b % 4, pbank=(2 * k, 2 * k + 1))
                pipe.push(job)
            groups5 = []
            for (wg_ap, wu_ap, wd_ap, eidx, F) in experts:
                nch = F // 128
                for c0 in range(0, nch, 4):
                    groups5.append((wg_ap, wu_ap, wd_ap, eidx, c0, min(4, nch - c0)))
            units5 = [(gi_, t4) for gi_ in range(len(groups5)) for t4 in range(4)]
            loaded5 = set()
            cnt5 = {"si": 0, "di": 0}

            def ensure_w(gi_):
                if gi_ in loaded5:
                    return
                loaded5.add(gi_)
                (wg_ap, wu_ap, wd_ap, eidx, c0, ncg) = groups5[gi_]
                k = gi_ % 2
                P.dma(wg[k][:, :, 0:ncg * 128], wg_ap[:, c0 * 128:(c0 + ncg) * 128].rearrange("(kc p) n -> p kc n", p=128),
                      writes=[r_wk[k]], eng="pool")
                P.dma(wu[k][:, :, 0:ncg * 128], wu_ap[:, c0 * 128:(c0 + ncg) * 128].rearrange("(kc p) n -> p kc n", p=128),
                      writes=[r_wk[k]], eng="pool")
                P.dma(wd[k][:, 0:ncg, :], wd_ap[c0 * 128:(c0 + ncg) * 128, :].rearrange("(c p) n -> p c n", p=128),
                      writes=[r_wk[k]], eng="pool")

            def emit_GU(u, c):
                gi_, t4 = units5[u]
                ncg = groups5[gi_][5]
                if c >= ncg:
                    return
                ensure_w(gi_)
                k = gi_ % 2
                a2 = u % 2
                s2 = cnt5["si"] % 2
                cnt5["si"] += 1
                bG, bU = 2 * s2, 2 * s2 + 1
                for kc in range(8):
                    mm(psb(bG), wg[k][:, kc, c * 128:(c + 1) * 128], hTh[:, kc, t4 * 512:(t4 + 1) * 512],
                       kc == 0, kc == 7, [r_wk[k], r_hThc[t4]], [r_ps[bG]])
                for kc in range(8):
                    mm(psb(bU), wu[k][:, kc, c * 128:(c + 1) * 128], hTh[:, kc, t4 * 512:(t4 + 1) * 512],
                       kc == 0, kc == 7, [r_wk[k], r_hThc[t4]], [r_ps[bU]])
                actf(sg[s2], psb(bG), AF.Silu, [r_ps[bG]], [r_sg[s2]])
                tt("dve", aT[a2][:, c, :], sg[s2], psb(bU), ALU.mult, [r_sg[s2], r_ps[bU]], [r_aT[a2]])

            def emit_D(u):
                gi_, t4 = units5[u]
                eidx, ncg = groups5[gi_][3], groups5[gi_][5]
                k = gi_ % 2
                a2 = u % 2
                for blk in range(4):
                    b = t4 * 4 + blk
                    for hh in range(2):
                        bank = 4 + 2 * (cnt5["di"] % 2) + hh
                        for c in range(ncg):
                            mm(psb(bank), aT[a2][:, c, blk * 128:(blk + 1) * 128], wd[k][:, c, hh * 512:(hh + 1) * 512],
                               c == 0, c == ncg - 1, [r_aT[a2], r_wk[k]], [r_ps[bank]])
                        sc_ = (1.0 / ALPHA) if eidx is None else comb[:, b, eidx:eidx + 1]
                        rd = [r_ps[bank], r_y[b]] + ([] if eidx is None else [r_comb])
                        stt("dve", yacc[:, b, hh * 512:(hh + 1) * 512], psb(bank), sc_, yacc[:, b, hh * 512:(hh + 1) * 512],
                            ALU.mult, ALU.add, rd, [r_y[b]])
                    cnt5["di"] += 1

            emit_GU(0, 0)
            for u in range(len(units5)):
                for c in range(1, 4):
                    emit_GU(u, c)
                if u + 1 < len(units5):
                    emit_GU(u + 1, 0)
                emit_D(u)
            for b in range(16):
                push_block(b)
            pipe.flush()
            P.barrier()
            A.release(m0)

        stage0()
        for l in range(L):
            if upto < 1 + 10 * l:
                break
            ml = A.mark()
            qT = A.bf16(4 * T).rearrange("p (a b) -> p a b", b=T)
            kT = A.bf16(4 * T).rearrange("p (a b) -> p a b", b=T)
            vsb = A.bf16(32 * 4 * 130).rearrange("p (a b c) -> p a b c", b=4, c=130)
            r_qT, r_kT, r_v = P.res("qT", multi=True), P.res("kT", multi=True), P.res("v", multi=True)
            stage1(l, qT, kT, vsb, r_qT, r_kT, r_v)
            if debug and upto == 1 + 10 * l:
                P.dma(dbg["qT"], qT.rearrange("p a b -> p (a b)"), reads=[r_qT])
                P.dma(dbg["kT"], kT.rearrange("p a b -> p (a b)"), reads=[r_kT])
                P.dma(dbg["v"], vsb.rearrange("p a b c -> p (a b c)"), reads=[r_v])
            if upto < 2 + 10 * l:
                break
            stage3(l)
            if upto < 3 + 10 * l:
                break
            stage2(l, qT, kT, vsb, r_qT, r_kT, r_v, 8 if l == 0 else 4)
            A.release(ml)
            bcl, r_bcl = load_bcl(l)
            if upto < 4 + 10 * l:
                break
            stage4(l, bcl, r_bcl, 8 if l == 0 else 4)
            if upto < 5 + 10 * l:
                break
            if l == 0:
                stage5(l, bcl, r_bcl, 0, False)
                stage5(l, bcl, r_bcl, TH, False)
            else:
                stage5(l, bcl, r_bcl, 0, True)
            P.barrier()
            A.release(ml)

        P.finalize(st)
        with nc.Block() as block:
            P.emit(block)
    return nc


def _rope_tables():
    half = 32
    inv = (1.0 / (10000.0 ** (np.arange(half, dtype=np.float32) * np.float32(2.0 / 64)))).astype(np.float32)
    pos = np.arange(T, dtype=np.float32)
    ang = pos[:, None] * inv[None, :]
    cos = np.cos(ang).astype(np.float32)
    sin = np.sin(ang).astype(np.float32)
    cs = np.zeros((128, 2, T), np.float32)
    for p in range(128):
        d = p % 64
        i = d % 32
        cs[p, 0] = cos[:, i]
        cs[p, 1] = -sin[:, i] if d < 32 else sin[:, i]
    return cs


def _prep_inputs(inp):
    f = lambda a: np.ascontiguousarray(np.asarray(a, dtype=np.float32))
    w_in = f(inp["w_in"])
    b_in = f(inp["b_in"])
    idx = np.arange(1024)
    blk = idx // 64
    d = idx % 64
    swap_idx = blk * 64 + (d + 32) % 64
    ext_cols = []
    for h in range(4):
        qh = np.arange(h * 128, (h + 1) * 128)
        kh = 512 + qh
        ext_cols += [qh, swap_idx[qh], kh, swap_idx[kh]]
    ext_cols.append(np.arange(1024, 4608))
    ext_cols = np.concatenate(ext_cols)
    w_in_ext = np.ascontiguousarray(w_in[:, :, ext_cols])
    b_ext = b_in[:, ext_cols]

    cs = _rope_tables()
    ident = np.eye(128, dtype=np.float32)
    bc0 = np.ascontiguousarray(np.broadcast_to(
        np.concatenate([f(inp["ln_in_g"]), f(inp["ln_in_b"])])[None, :], (128, 2 * D)))

    def bcl_for():
        rows = []
        for l in range(L):
            lam4 = np.concatenate([f(inp["lam_q1"])[l], f(inp["lam_k1"])[l], f(inp["lam_q2"])[l], f(inp["lam_k2"])[l]])
            row = np.concatenate([f(inp["ln1_g"])[l], f(inp["ln1_b"])[l], f(inp["ln2_g"])[l], f(inp["ln2_b"])[l],
                                  f(inp["b_o"])[l], b_in[l, 1024:1536], f(inp["subln_g"])[l], lam4,
                                  f(inp["moe_br"])[0]])
            assert row.shape[0] == BCL
            rows.append(np.broadcast_to(row[None, :], (128, BCL)))
        return np.ascontiguousarray(np.stack(rows))
    bcl = bcl_for()

    conv_w = f(inp["conv_w"])
    conv_b = f(inp["conv_b"])
    rg_wa, rg_wx = f(inp["rg_wa"]), f(inp["rg_wx"])
    rg_ba, rg_bx, rg_lam = f(inp["rg_ba"]), f(inp["rg_bx"]), f(inp["rg_lam"])

    def per_parity(rev):
        colp = np.zeros((128, L * NCOLP), np.float32)
        rgbd = np.zeros((L, 16, 128, 128), np.float32)
        for l in range(L):
            base = l * NCOLP
            colp[:, base:base + 44] = b_ext[l].reshape(44, 128).T
            w5 = np.zeros((5, 512), np.float32)
            if not rev:
                w5[0:4] = conv_w[l]
            else:
                w5[1:5] = conv_w[l][::-1]
            for j in range(5):
                colp[:, base + 44 + j * 4:base + 44 + (j + 1) * 4] = w5[j].reshape(4, 128).T
            colp[:, base + 64:base + 68] = conv_b[l].reshape(4, 128).T
            for dd in range(2):
                src = (1 - dd) if rev else dd
                colp[:, base + 68 + dd * 4:base + 72 + dd * 4] = rg_ba[l, src].reshape(4, 128).T
                colp[:, base + 76 + dd * 4:base + 80 + dd * 4] = rg_bx[l, src].reshape(4, 128).T
                colp[:, base + 84 + dd * 4:base + 88 + dd * 4] = rg_lam[l, src].reshape(4, 128).T
                for ax, wsrc in enumerate((rg_wa, rg_wx)):
                    for c in range(4):
                        mtx = rgbd[l, dd * 8 + ax * 4 + c]
                        mtx[0:64, 0:64] = wsrc[l, src, 2 * c]
                        mtx[64:128, 64:128] = wsrc[l, src, 2 * c + 1]
        return colp, rgbd

    par = [per_parity(False), per_parity(True)]
    cs_rev = np.ascontiguousarray(cs[:, :, ::-1])
    x = f(inp["x"])
    shared = {
        "bc0": bc0, "bcl": bcl, "ident": ident, "w_in_ext": w_in_ext,
        "w_pa": f(inp["w_pa"]), "w_pb": f(inp["w_pb"]), "w_o": f(inp["w_o"]),
        "ffn_wg": f(inp["ffn_wg"]), "ffn_wu": f(inp["ffn_wu"]), "ffn_wd": f(inp["ffn_wd"]),
        "moe_wr": f(inp["moe_wr"]), "moe_wg": f(inp["moe_wg"]), "moe_wu": f(inp["moe_wu"]),
        "moe_wd": f(inp["moe_wd"]),
    }
    in_maps = []
    for c in range(8):
        b, rev = c // 2, c % 2
        m = dict(shared)
        m["x"] = np.ascontiguousarray(x[b, ::-1]) if rev else np.ascontiguousarray(x[b])
        m["cs"] = cs_rev if rev else cs
        m["colp"], m["rg_bd"] = par[rev]
        in_maps.append(m)
    return in_maps


_NC_CACHE = {}


def kernel(**inputs):
    in_maps = _prep_inputs(inputs)
    if "nc" not in _NC_CACHE:
        _NC_CACHE["nc"] = build_program()
    nc = _NC_CACHE["nc"]
    res = run_bass_kernel_spmd(nc, in_maps, core_ids=list(range(8)))
    out = np.zeros((4, T, D), np.float32)
    for c in range(8):
        o = np.asarray(res.results[c]["out"], dtype=np.float32)
        b, rev = c // 2, c % 2
        if rev:
            out[b, TH:] = o[::-1]
        else:
            out[b, :TH] = o
    return out
```
